# Optimizing a Trainium2 kernel written in Bass

```python
import math
import jax, jax.numpy as jnp
from jax import lax
import numpy as np

D_MODEL = 1024
BATCH = 8
SEQ = 4096
DEPTH = 1

HEAD_DIM = 64
MIX_WIDTH = D_MODEL
N_HEADS = MIX_WIDTH // HEAD_DIM
N_HEADS_B = N_HEADS // 4
N_HEADS_A = N_HEADS - N_HEADS_B
WIDTH_A = N_HEADS_A * HEAD_DIM
WIDTH_B = N_HEADS_B * HEAD_DIM
DILATED_PATTERNS = ((128, 1), (512, 4), (2048, 16))
DIL_BLOCK = 128
MOBA_BLOCK = 256
MOBA_TOPK = 3
MOBA_QCHUNK = 64
N_MEM = 256
CROSS_HEADS = 4
CROSS_HEAD_DIM = D_MODEL // CROSS_HEADS
D_FF = ((8 * D_MODEL // 3 + 255) // 256) * 256
REL_BUCKETS = 32
REL_MAX_DIST = 2048
NORM_EPS = 1e-6
NEG_INF = -jnp.inf

kernel_name = "hymba_style_dilated_moba_macaron_block"


def rms_norm(x, g):
    xf = x.astype(jnp.float32)
    y = xf * lax.rsqrt(jnp.mean(xf * xf, axis=-1, keepdims=True) + NORM_EPS)
    return (y * g.astype(jnp.float32)).astype(x.dtype)


def swiglu(x, w_gu, w_down):
    gu = x @ w_gu
    g, u = jnp.split(gu, 2, axis=-1)
    return (jax.nn.silu(g) * u) @ w_down


def rel_bucket(dist):
    max_exact = REL_BUCKETS // 2
    n = jnp.maximum(dist, 0)
    nf = jnp.maximum(n, 1).astype(jnp.float32)
    large = max_exact + (jnp.log(nf / max_exact) / math.log(REL_MAX_DIST / max_exact)
                         * (REL_BUCKETS - max_exact)).astype(jnp.int32)
    large = jnp.minimum(large, REL_BUCKETS - 1)
    return jnp.where(n < max_exact, n, large)


def split_heads(a, n_heads, head_dim):
    b, s, _ = a.shape
    return a.reshape(b, s, n_heads, head_dim).transpose(0, 2, 1, 3)


def merge_heads(a):
    b, h, s, e = a.shape
    return a.transpose(0, 2, 1, 3).reshape(b, s, h * e)


def dilated_window_pattern(q, k, v, bias_tab, window, dilation):
    B, H, S, E = q.shape
    n_steps = window // dilation
    L = S // dilation
    nblk = -(-L // DIL_BLOCK)
    Lp = nblk * DIL_BLOCK

    def to_sub(a):
        a = a.reshape(B, H, L, dilation, E).transpose(0, 1, 3, 2, 4)
        a = jnp.pad(a, ((0, 0), (0, 0), (0, 0), (0, Lp - L), (0, 0)))
        return a.reshape(B, H, dilation, nblk, DIL_BLOCK, E)

    def band(a):
        prev = jnp.pad(a, ((0, 0), (0, 0), (0, 0), (1, 0), (0, 0), (0, 0)))[:, :, :, :-1]
        return jnp.concatenate([prev, a], axis=4)

    qs, ks, vs = to_sub(q), to_sub(k), to_sub(v)
    kb, vb = band(ks), band(vs)
    scale = HEAD_DIM ** -0.5
    s = jnp.einsum('bhrnqe,bhrnke->bhrnqk', qs, kb).astype(jnp.float32) * scale
    i = jnp.arange(DIL_BLOCK)[:, None]
    j = jnp.arange(2 * DIL_BLOCK)[None, :]
    delta = DIL_BLOCK + i - j
    bias = bias_tab[:, rel_bucket(delta * dilation)].astype(jnp.float32)
    blk = jnp.arange(nblk)[:, None, None]
    valid = (delta >= 0) & (delta <= n_steps) & ((blk > 0) | (j >= DIL_BLOCK))
    s = jnp.where(valid, s + bias[None, :, None, None], NEG_INF)
    m = jnp.max(s, axis=-1, keepdims=True)
    p = jnp.exp(s - m)
    l = jnp.sum(p, axis=-1, keepdims=True)
    o = jnp.einsum('bhrnqk,bhrnke->bhrnqe', p, vb.astype(jnp.float32)) / l

    def from_sub(a):
        f = a.shape[-1]
        a = a.reshape(B, H, dilation, Lp, f)[:, :, :, :L]
        return a.transpose(0, 1, 3, 2, 4).reshape(B, H, S, f)

    return from_sub(o), from_sub(m), from_sub(l)


def dilated_attention(q, k, v, bias_tab):
    outs = [dilated_window_pattern(q, k, v, bias_tab, w, d) for (w, d) in DILATED_PATTERNS]
    m_all = outs[0][1]
    for _, m_i, _ in outs[1:]:
        m_all = jnp.maximum(m_all, m_i)
    num = 0.0
    den = 0.0
    for o_i, m_i, l_i in outs:
        w_i = l_i * jnp.exp(m_i - m_all)
        num = num + w_i * o_i
        den = den + w_i
    return num / den


def moba_attention(q, k, v, bias_tab):
    B, H, S, E = q.shape
    BS = MOBA_BLOCK
    nb = -(-S // BS)
    Sp = nb * BS
    pad = lambda a: jnp.pad(a, ((0, 0), (0, 0), (0, Sp - S), (0, 0)))
    qp, kp, vp = pad(q), pad(k), pad(v)
    kb = kp.reshape(B, H, nb, BS, E)
    vb = vp.reshape(B, H, nb, BS, E)
    kmean = jnp.mean(kb.astype(jnp.float32), axis=3)
    gate = jnp.einsum('bhse,bhne->bhsn', qp.astype(jnp.float32), kmean)
    qblk = jnp.arange(Sp) // BS
    past = jnp.arange(nb)[None, :] < qblk[:, None]
    gate = jnp.where(past, gate, NEG_INF)
    top = min(MOBA_TOPK, nb)
    _, sel = lax.top_k(gate, top)
    nc = Sp // MOBA_QCHUNK
    chunk = lambda a: jnp.moveaxis(a.reshape(B, H, nc, MOBA_QCHUNK, *a.shape[3:]), 2, 0)
    gather = jax.vmap(jax.vmap(lambda tab, ix: tab[ix]))
    head_idx = jnp.arange(H)[:, None, None, None]
    scale = HEAD_DIM ** -0.5

    def one_chunk(args):
        qc, selc, c = args
        t = c * MOBA_QCHUNK + jnp.arange(MOBA_QCHUNK)
        own = (c * MOBA_QCHUNK) // BS
        k_own = lax.dynamic_index_in_dim(kb, own, axis=2, keepdims=False)
        v_own = lax.dynamic_index_in_dim(vb, own, axis=2, keepdims=False)
        k_sel = gather(kb, selc)
        v_sel = gather(vb, selc)
        s_sel = jnp.einsum('bhqe,bhqjke->bhqjk', qc, k_sel).astype(jnp.float32) * scale
        key_pos = selc[..., None] * BS + jnp.arange(BS)
        s_sel = s_sel + bias_tab[head_idx, rel_bucket(t[:, None, None] - key_pos)].astype(jnp.float32)
        sel_valid = jnp.arange(top)[None, :] < (t // BS)[:, None]
        s_sel = jnp.where(sel_valid[:, :, None], s_sel, NEG_INF)
        s_own = jnp.einsum('bhqe,bhke->bhqk', qc, k_own).astype(jnp.float32) * scale
        dist_own = t[:, None] - (own * BS + jnp.arange(BS))[None, :]
        s_own = s_own + bias_tab[:, rel_bucket(dist_own)].astype(jnp.float32)[None]
        s_own = jnp.where(dist_own >= 0, s_own, NEG_INF)
        logits = jnp.concatenate([s_sel.reshape(B, H, MOBA_QCHUNK, top * BS), s_own], axis=-1)
        p = jax.nn.softmax(logits, axis=-1)
        p_sel = p[..., :top * BS].reshape(B, H, MOBA_QCHUNK, top, BS)
        p_own = p[..., top * BS:]
        return (jnp.einsum('bhqjk,bhqjke->bhqe', p_sel, v_sel.astype(jnp.float32))
                + jnp.einsum('bhqk,bhke->bhqe', p_own, v_own.astype(jnp.float32)))

    o = lax.map(one_chunk, (chunk(qp), chunk(sel), jnp.arange(nc)))
    o = jnp.moveaxis(o, 0, 2).reshape(B, H, Sp, E)[:, :, :S]
    return o


def memory_cross_attention(x, mem, g_x, g_mem, w_cq, w_ckv, w_co):
    B, S, _ = x.shape
    h = rms_norm(x, g_x)
    mn = rms_norm(mem, g_mem)
    q = (h @ w_cq).reshape(B, S, CROSS_HEADS, CROSS_HEAD_DIM)
    kv = mn @ w_ckv
    k, v = jnp.split(kv, 2, axis=-1)
    k = k.reshape(B, mem.shape[1], CROSS_HEADS, CROSS_HEAD_DIM)
    v = v.reshape(B, mem.shape[1], CROSS_HEADS, CROSS_HEAD_DIM)
    s = jnp.einsum('bshe,bmhe->bhsm', q, k).astype(jnp.float32) * CROSS_HEAD_DIM ** -0.5
    p = jax.nn.softmax(s, axis=-1)
    o = jnp.einsum('bhsm,bmhe->bshe', p, v.astype(jnp.float32)).reshape(B, S, D_MODEL)
    return o.astype(x.dtype) @ w_co


def setup_inputs(seed: int = 0) -> dict:
    key = jax.random.key(seed)
    ks = jax.random.split(key, 24)
    f32 = jnp.float32

    def w(k, shape, fan_in):
        return jax.random.normal(k, shape, f32) * fan_in ** -0.5

    def gain(k, shape):
        return 1.0 + 0.05 * jax.random.normal(k, shape, f32)

    L = DEPTH
    return {
        "x": jax.random.normal(ks[0], (BATCH, SEQ, D_MODEL), f32),
        "mem": jax.random.normal(ks[1], (BATCH, N_MEM, D_MODEL), f32),
        "g_ffn1": gain(ks[2], (L, D_MODEL)),
        "w_ffn1_gu": w(ks[3], (L, D_MODEL, 2 * D_FF), D_MODEL),
        "w_ffn1_down": w(ks[4], (L, D_FF, D_MODEL), D_FF),
        "g_mix": gain(ks[5], (L, D_MODEL)),
        "w_in": w(ks[6], (L, D_MODEL, 3 * MIX_WIDTH), D_MODEL),
        "rel_bias": 0.5 * jax.random.normal(ks[7], (REL_BUCKETS, N_HEADS), f32),
        "g_out_a": gain(ks[8], (L, WIDTH_A)),
        "g_out_b": gain(ks[9], (L, WIDTH_B)),
        "w_out": w(ks[10], (L, MIX_WIDTH, D_MODEL), MIX_WIDTH),
        "g_cross": gain(ks[11], (L, D_MODEL)),
        "g_mem": gain(ks[12], (L, D_MODEL)),
        "w_cq": w(ks[13], (L, D_MODEL, D_MODEL), D_MODEL),
        "w_ckv": w(ks[14], (L, D_MODEL, 2 * D_MODEL), D_MODEL),
        "w_co": w(ks[15], (L, D_MODEL, D_MODEL), D_MODEL),
        "g_ffn2": gain(ks[16], (L, D_MODEL)),
        "w_ffn2_gu": w(ks[17], (L, D_MODEL, 2 * D_FF), D_MODEL),
        "w_ffn2_down": w(ks[18], (L, D_FF, D_MODEL), D_FF),
        "g_final": gain(ks[19], (D_MODEL,)),
    }


def reference(x, mem, g_ffn1, w_ffn1_gu, w_ffn1_down, g_mix, w_in, rel_bias, g_out_a, g_out_b,
              w_out, g_cross, g_mem, w_cq, w_ckv, w_co, g_ffn2, w_ffn2_gu, w_ffn2_down, g_final):
    bias_a = rel_bias[:, :N_HEADS_A].T
    bias_b = rel_bias[:, N_HEADS_A:].T
    for layer in range(DEPTH):
        x = x + 0.5 * swiglu(rms_norm(x, g_ffn1[layer]), w_ffn1_gu[layer], w_ffn1_down[layer])
        h = rms_norm(x, g_mix[layer])
        proj = h @ w_in[layer]
        qa, ka, va, qb, kb, vb = jnp.split(
            proj, np.cumsum([WIDTH_A, WIDTH_A, WIDTH_A, WIDTH_B, WIDTH_B]).tolist(), axis=-1)
        oa = dilated_attention(split_heads(qa, N_HEADS_A, HEAD_DIM), split_heads(ka, N_HEADS_A, HEAD_DIM),
                               split_heads(va, N_HEADS_A, HEAD_DIM), bias_a)
        ob = moba_attention(split_heads(qb, N_HEADS_B, HEAD_DIM), split_heads(kb, N_HEADS_B, HEAD_DIM),
                            split_heads(vb, N_HEADS_B, HEAD_DIM), bias_b)
        ya = rms_norm(merge_heads(oa).astype(x.dtype), g_out_a[layer])
        yb = rms_norm(merge_heads(ob).astype(x.dtype), g_out_b[layer])
        x = x + jnp.concatenate([ya, yb], axis=-1) @ w_out[layer]
        x = x + memory_cross_attention(x, mem, g_cross[layer], g_mem[layer], w_cq[layer], w_ckv[layer], w_co[layer])
        x = x + 0.5 * swiglu(rms_norm(x, g_ffn2[layer]), w_ffn2_gu[layer], w_ffn2_down[layer])
    return rms_norm(x, g_final)
```

```python
import math
from contextlib import ExitStack

import numpy as np
import concourse.bass as bass
import concourse.mybir as mybir
from concourse.bass_utils import run_bass_kernel_spmd

F32 = mybir.dt.float32
BF16 = mybir.dt.bfloat16
AF = mybir.ActivationFunctionType
ALU = mybir.AluOpType
AX = mybir.AxisListType

S = 4096
D = 1024
DFF = 2816
NT = S // 128
NW = 2944
WOFF = 384
EPS = 1e-6
NCORES = 8


class Op:
    __slots__ = ("eng", "fn", "deps", "dma", "needs", "sem", "val")


class Sched:
    ENG = ("pe", "act", "dve", "pool", "sp")

    def __init__(self, nc, es):
        self.nc = nc
        self.es = es
        self.ops = {e: [] for e in self.ENG}
        self.tok = {}
        self.dma_last = {}
        self.dma_cnt = {}
        self.dma_sem = {}
        self.eng_sem = {}
        self.eng_cnt = {e: 0 for e in self.ENG}
        self.tail = {e: None for e in self.ENG}
        self.waited = {e: {} for e in self.ENG}
        self.barrier_deps = []
        for e in ("pe", "act", "dve", "pool"):
            self.eng_sem[e] = es.enter_context(nc.semaphore("sem_" + e))

    def _dsem(self, key):
        if key not in self.dma_sem:
            self.dma_sem[key] = self.es.enter_context(self.nc.semaphore("dsem_%d" % len(self.dma_sem)))
            self.dma_cnt[key] = 0
        return self.dma_sem[key]

    def op(self, eng, fn, r=(), w=(), dma=None):
        o = Op()
        o.eng, o.fn, o.dma, o.needs, o.deps = eng, fn, dma, False, []
        o.sem, o.val = None, 0
        deps = {}

        def add(d):
            if d is not None and d is not o:
                deps[id(d)] = d

        for t in r:
            st = self.tok.get(t)
            if st:
                add(st[0])
        for t in w:
            st = self.tok.get(t)
            if st:
                add(st[0])
                for rd in st[1].values():
                    add(rd)
                for rd in st[2]:
                    add(rd)
        if dma is not None:
            add(self.dma_last.get(dma))
            self.dma_last[dma] = o
            self._dsem(dma)
        for t in r:
            st = self.tok.setdefault(t, [None, {}, []])
            if dma is None:
                st[1][eng] = o
            else:
                st[2].append(o)
        for t in w:
            self.tok[t] = [o, {}, []]
        for d in deps.values():
            if d.dma is None and dma is None and d.eng == eng and eng == "pe":
                continue
            d.needs = True
            o.deps.append(d)
        self.ops[eng].append(o)
        if dma is None:
            self.tail[eng] = o
        return o

    def emit_stage(self):
        nc = self.nc
        for e in self.ENG:
            for o in self.ops[e]:
                if o.dma is not None:
                    self.dma_cnt[o.dma] += 1
                    o.sem = self.dma_sem[o.dma]
                    o.val = 16 * self.dma_cnt[o.dma]
                elif o.needs or o is self.tail[e]:
                    o.needs = True
                    self.eng_cnt[e] += 1
                    o.sem = self.eng_sem[e]
                    o.val = self.eng_cnt[e]
        bdeps = list(self.barrier_deps)

        def run(ename, eobj):
            waited = self.waited[ename]

            def wait_for(pairs):
                best = {}
                for sem, val in pairs:
                    k = id(sem)
                    if k not in best or best[k][1] < val:
                        best[k] = (sem, val)
                for k, (sem, val) in best.items():
                    if waited.get(k, 0) >= val:
                        continue
                    eobj.wait_ge(sem, val)
                    waited[k] = val

            wait_for(bdeps)
            for o in self.ops[ename]:
                wait_for([(d.sem, d.val) for d in o.deps])
                ins = o.fn(eobj)
                if o.dma is not None:
                    ins.then_inc(o.sem, 16)
                elif o.needs:
                    ins.then_inc(o.sem, 1)

        with nc.Block() as block:
            @block.tensor
            def _(e):
                run("pe", e)

            @block.scalar
            def _(e):
                run("act", e)

            @block.vector
            def _(e):
                run("dve", e)

            @block.gpsimd
            def _(e):
                run("pool", e)

            @block.sync
            def _(e):
                run("sp", e)

        nb = []
        for e in ("pe", "act", "dve", "pool"):
            if self.eng_cnt[e] > 0:
                nb.append((self.eng_sem[e], self.eng_cnt[e]))
        for k, sem in self.dma_sem.items():
            if self.dma_cnt[k] > 0:
                nb.append((sem, 16 * self.dma_cnt[k]))
        self.barrier_deps = nb
        self.ops = {e: [] for e in self.ENG}
        self.tok = {}
        self.dma_last = {}
        self.tail = {e: None for e in self.ENG}

    def final_wait(self):
        nc = self.nc
        bdeps = list(self.barrier_deps)
        with nc.Block() as block:
            @block.sync
            def _(e):
                for sem, val in bdeps:
                    e.wait_ge(sem, val)

            @block.gpsimd
            def _(e):
                for sem, val in bdeps:
                    e.wait_ge(sem, val)


class Ctx:
    pass


_PSN = [0]


def _psum_banks(nc, es, n_f32, n_bf16):
    _PSN[0] += 1
    u = _PSN[0]
    pf = [es.enter_context(nc.psum_tensor("pf%d_%d" % (u, i), [128, 512], F32)) for i in range(n_f32)]
    pb = [es.enter_context(nc.psum_tensor("pb%d_%d" % (u, i), [128, 1024], BF16)) for i in range(n_bf16)]
    return pf, pb


def _load_gain(sch, C, dst, row, key):
    src = bass.AP(C.gall_t, row * D, [[0, 128], [1, D]])
    sch.op("sp", lambda e: e.dma_start(out=dst[:, :], in_=src), w=[("g", key)], dma="g" + key)


def _norm_to_hn(sch, C, xap, xtok, gB, gkey, hn_ap, hntok, k):
    st = C.st
    b = (k % 4) * 3
    sa, sb, sc = st[:, b:b + 1], st[:, b + 1:b + 2], st[:, b + 2:b + 3]
    tk = ("st", k % 4)
    sch.op("act", lambda e: e.activation(out=hn_ap, in_=xap, func=AF.Square, accum_out=sa),
           r=[xtok], w=[hntok, tk])
    sch.op("dve", lambda e: e.tensor_scalar(out=sb, in0=sa, scalar1=1.0 / D, scalar2=EPS,
                                            op0=ALU.mult, op1=ALU.add), r=[tk], w=[tk])
    sch.op("pool", lambda e: e.tensor_tensor(out=sc, in0=sb, in1=C.mhalf[:, 0:1], op=ALU.pow),
           r=[tk, ("mhalf",)], w=[tk])
    sch.op("dve", lambda e: e.scalar_tensor_tensor(out=hn_ap, in0=xap, scalar=sc, in1=gB[:, :],
                                                   op0=ALU.mult, op1=ALU.mult),
           r=[xtok, tk, ("g", gkey)], w=[hntok])


def _transpose_to(sch, C, hn_ap_fn, hntok, pb, pbi, dstT, dsttok, col0, evac_eng="act"):
    ps = pb[pbi]
    ptok = ("pb", pbi)
    for cc in range(8):
        sch.op("pe", lambda e, cc=cc: e.transpose(out=ps[:, cc * 128:(cc + 1) * 128],
                                                  in_=hn_ap_fn(cc), identity=C.ident[:, :]),
               r=[hntok, ("ident",)], w=[ptok])
    src = ps[:, :].rearrange("p (c t) -> p c t", c=8)
    dst = dstT[:, :, col0:col0 + 128]
    if evac_eng == "act":
        sch.op("act", lambda e: e.copy(out=dst, in_=src), r=[ptok], w=[dsttok])
    else:
        sch.op("dve", lambda e: e.tensor_copy(out=dst, in_=src), r=[ptok], w=[dsttok])


def _load_weight_cols(sch, dst3, src2, c0, c1, tok, keyi, dst_c0=None):
    if dst_c0 is None:
        dst_c0 = c0
    srcv = src2.rearrange("(kc p) n -> p kc n", p=128)[:, :, c0:c1]
    dstv = dst3[:, :, dst_c0:dst_c0 + (c1 - c0)]
    sch.op("pool", lambda e: e.dma_start(out=dstv, in_=srcv), w=[tok], dma="w%d" % (keyi % 8))


def ffn_stage(nc, sch, C, name, xin, xout, wgu_d, wdn_d, grow, frow):
    with ExitStack() as es:
        T = lambda n, shp, dt: es.enter_context(nc.sbuf_tensor(name + n, shp, dt))
        wgu = T("wgu", [128, 8, 2 * DFF], BF16)
        wdn = T("wdn", [128, 22, D], BF16)
        gB = T("gB", [128, D], F32)
        gF = T("gF", [128, D], F32) if frow is not None else None
        xt = T("xt", [128, 4, D], F32)
        hn = T("hn", [128, 4, D], BF16)
        hT = T("hT", [128, 8, 512], BF16)
        aT = T("aT", [128, 22, 512], BF16)
        sg = T("sg", [128, 2, 512], F32)
        pf, pb = _psum_banks(nc, es, 6, 2)
        psA, psB, psC = pf[0:2], pf[2:4], pf[4:6]

        _load_gain(sch, C, gB, grow, "B")
        if frow is not None:
            _load_gain(sch, C, gF, frow, "F")
        def load_weights():
            ki = 0
            for j in range(4):
                _load_weight_cols(sch, wgu, wgu_d, j * 704, (j + 1) * 704, ("wgu", j), ki); ki += 1
                _load_weight_cols(sch, wgu, wgu_d, DFF + j * 704, DFF + (j + 1) * 704, ("wgu", j), ki); ki += 1
            for j in range(11):
                srcv = wdn_d.rearrange("(fc p) n -> p fc n", p=128)[:, 2 * j:2 * j + 2, :]
                dstv = wdn[:, 2 * j:2 * j + 2, :]
                sch.op("pool", lambda e, dstv=dstv, srcv=srcv: e.dma_start(out=dstv, in_=srcv),
                       w=[("wdn", j)], dma="w%d" % (ki % 8)); ki += 1

        cnt = {"x": 0, "k": 0}

        def load_x(src_d, t):
            slot = cnt["x"] % 4
            cnt["x"] += 1
            sch.op("sp", lambda e: e.dma_start(out=xt[:, slot, :], in_=src_d[t * 128:(t + 1) * 128, :]),
                   w=[("xt", slot)], dma="xl%d" % slot)
            return slot

        def phaseA_norm(ci):
            for s in range(4):
                t = ci * 4 + s
                slot = load_x(xin, t)
                k = ci * 4 + s
                hnap = hn[:, k % 4, :]
                _norm_to_hn(sch, C, xt[:, slot, :], ("xt", slot), gB, "B", hnap, ("hn", k % 4), k)

        def phaseA_tr(ci):
            for s in range(4):
                k = ci * 4 + s
                _transpose_to(sch, C, lambda cc, k=k: hn[:, k % 4, cc * 128:(cc + 1) * 128], ("hn", k % 4),
                              pb, k % 2, hT, ("hT",), s * 128)

        def phaseB(ci):
            for fi in range(22):
                pa, pbk = psA[fi % 2], psB[fi % 2]
                ta, tb_ = ("pf", fi % 2), ("pf", 2 + fi % 2)
                for cc in range(8):
                    sch.op("pe", lambda e, cc=cc, pa=pa, fi=fi: e.matmul(
                        pa[:, :], lhsT=wgu[:, cc, fi * 128:(fi + 1) * 128], rhs=hT[:, cc, :],
                        start=(cc == 0), stop=(cc == 7)),
                           r=[("wgu", (fi * 128) // 704), ("wgu", (fi * 128 + 127) // 704), ("hT",)], w=[ta])
                for cc in range(8):
                    sch.op("pe", lambda e, cc=cc, pbk=pbk, fi=fi: e.matmul(
                        pbk[:, :], lhsT=wgu[:, cc, DFF + fi * 128:DFF + (fi + 1) * 128], rhs=hT[:, cc, :],
                        start=(cc == 0), stop=(cc == 7)),
                           r=[("wgu", (fi * 128) // 704), ("wgu", (fi * 128 + 127) // 704), ("hT",)], w=[tb_])
                sgap = sg[:, fi % 2, :]
                sch.op("act", lambda e, pa=pa, sgap=sgap: e.activation(out=sgap, in_=pa[:, :], func=AF.Silu),
                       r=[ta], w=[("sg", fi % 2)])
                sch.op("dve", lambda e, pbk=pbk, sgap=sgap, fi=fi: e.tensor_tensor(
                    out=aT[:, fi, :], in0=sgap, in1=pbk[:, :], op=ALU.mult),
                       r=[("sg", fi % 2), tb_], w=[("aT", fi)])

        def phaseC(ci):
            for s in range(4):
                t = ci * 4 + s
                slot = load_x(xin, t)
                xr = xt[:, slot, :]
                for half in range(2):
                    pc = psC[half]
                    tc_ = ("pf", 4 + half)
                    for fi in range(22):
                        sch.op("pe", lambda e, fi=fi, pc=pc, half=half, s=s: e.matmul(
                            pc[:, :], lhsT=aT[:, fi, s * 128:(s + 1) * 128],
                            rhs=wdn[:, fi, half * 512:(half + 1) * 512],
                            start=(fi == 0), stop=(fi == 21)), r=[("aT", fi), ("wdn", fi // 2)], w=[tc_])
                    xh = xt[:, slot, half * 512:(half + 1) * 512]
                    sch.op("dve", lambda e, pc=pc, xh=xh: e.scalar_tensor_tensor(
                        out=xh, in0=pc[:, :], scalar=0.5, in1=xh, op0=ALU.mult, op1=ALU.add),
                           r=[tc_, ("xt", slot)], w=[("xt", slot)])
                if frow is not None:
                    k = cnt["k"]; cnt["k"] += 1
                    b = (k % 4) * 3
                    sa, sb, sc = C.st[:, b:b + 1], C.st[:, b + 1:b + 2], C.st[:, b + 2:b + 3]
                    tk = ("st", k % 4)
                    junk = sg[:, :, :].rearrange("p a b -> p (a b)")
                    sch.op("act", lambda e, xr=xr, sa=sa, junk=junk: e.activation(
                        out=junk, in_=xr, func=AF.Square, accum_out=sa),
                           r=[("xt", slot)], w=[("sg", 0), ("sg", 1), tk])
                    sch.op("dve", lambda e, sa=sa, sb=sb: e.tensor_scalar(
                        out=sb, in0=sa, scalar1=1.0 / D, scalar2=EPS, op0=ALU.mult, op1=ALU.add), r=[tk], w=[tk])
                    sch.op("pool", lambda e, sb=sb, sc=sc: e.tensor_tensor(
                        out=sc, in0=sb, in1=C.mhalf[:, 0:1], op=ALU.pow), r=[tk, ("mhalf",)], w=[tk])
                    sch.op("dve", lambda e, xr=xr, sc=sc: e.scalar_tensor_tensor(
                        out=xr, in0=xr, scalar=sc, in1=gF[:, :], op0=ALU.mult, op1=ALU.mult),
                           r=[("xt", slot), tk, ("g", "F")], w=[("xt", slot)])
                sch.op("sp", lambda e, xr=xr, t=t: e.dma_start(out=xout[t * 128:(t + 1) * 128, :], in_=xr),
                       r=[("xt", slot)], w=[("xout", t)], dma="xs%d" % slot)

        cnt["k"] = 64
        phaseA_norm(0)
        load_weights()
        phaseA_tr(0)
        if True:
            phaseA_norm(1)
        phaseB(0)
        for ci in range(8):
            if ci + 1 < 8:
                phaseA_tr(ci + 1)
            phaseC(ci)
            if ci + 2 < 8:
                phaseA_norm(ci + 2)
            if ci + 1 < 8:
                phaseB(ci + 1)
        sch.emit_stage()


def inproj_stage(nc, sch, C):
    with ExitStack() as es:
        T = lambda n, shp, dt: es.enter_context(nc.sbuf_tensor("ip" + n, shp, dt))
        win = T("win", [128, 8, 3072], BF16)
        gB = T("gB", [128, D], F32)
        xt = T("xt", [128, 4, D], F32)
        hn = T("hn", [128, 4, D], BF16)
        hT = T("hT", [128, 8, 512], BF16)
        qk = T("qk", [128, 2, 22, 512], BF16)
        vt = T("vt", [128, 2, 4, 4, 65], BF16)
        pf, pb = _psum_banks(nc, es, 6, 2)

        _load_gain(sch, C, gB, 1, "B")
        sch.op("pool", lambda e: e.memset(vt[:, :, :, :, 64:65], 1.0), w=[("vt", 0), ("vt", 1)])

        fcs = [0, 1, 2, 3, 4, 5, 18, 19, 6, 7, 8, 9, 10, 11, 20, 21, 12, 13, 14, 15, 16, 17]
        qTv = C.qT.rearrange("(i p) t -> p i t", p=128)
        kTv = C.kT.rearrange("(i p) t -> p i t", p=128)
        vTv = C.vT.rearrange("(i p) t -> p i t", p=128)
        Vv = C.V.rearrange("h p t e -> p h t e")
        cnt = {"x": 0, "k": 0, "pf": 0}

        def phaseA_norm(ci):
            for s in range(4):
                t = ci * 4 + s
                slot = cnt["x"] % 4; cnt["x"] += 1
                sch.op("sp", lambda e, slot=slot, t=t: e.dma_start(out=xt[:, slot, :],
                                                                  in_=C.x1[t * 128:(t + 1) * 128, :]),
                       w=[("xt", slot)], dma="xl%d" % slot)
                k = ci * 4 + s
                _norm_to_hn(sch, C, xt[:, slot, :], ("xt", slot), gB, "B", hn[:, k % 4, :], ("hn", k % 4), k)

        def phaseA_tr(ci):
            for s in range(4):
                k = ci * 4 + s
                _transpose_to(sch, C, lambda cc, k=k: hn[:, k % 4, cc * 128:(cc + 1) * 128], ("hn", k % 4),
                              pb, k % 2, hT, ("hT",), s * 128)

        def phaseB(ci):
            sl = ci % 2
            for i, fc in enumerate(fcs):
                bi = cnt["pf"] % 6; cnt["pf"] += 1
                ps = pf[bi]
                for cc in range(8):
                    sch.op("pe", lambda e, cc=cc, ps=ps, fc=fc: e.matmul(
                        ps[:, :], lhsT=win[:, cc, fc * 128:(fc + 1) * 128], rhs=hT[:, cc, :],
                        start=(cc == 0), stop=(cc == 7)), r=[("win", fc // 2), ("hT",)], w=[("pf", bi)])
                dst = qk[:, sl, i, :]
                if i % 2 == 0:
                    sch.op("act", lambda e, ps=ps, dst=dst: e.copy(out=dst, in_=ps[:, :]),
                           r=[("pf", bi)], w=[("qk", sl, i)])
                else:
                    sch.op("dve", lambda e, ps=ps, dst=dst: e.tensor_copy(out=dst, in_=ps[:, :]),
                           r=[("pf", bi)], w=[("qk", sl, i)])
            for (dv, base, lo, n, nm) in ((qTv, 0, 0, 4, "q0"), (qTv, 0, 4, 4, "q1"), (kTv, 8, 0, 4, "k0"),
                                          (kTv, 8, 4, 4, "k1"), (vTv, 16, 0, 3, "v0"), (vTv, 16, 3, 3, "v1")):
                sch.op("sp", lambda e, sl=sl, ci=ci, dv=dv, base=base, lo=lo, n=n: e.dma_start(
                    out=dv[:, lo:lo + n, ci * 512:(ci + 1) * 512], in_=qk[:, sl, base + lo:base + lo + n, :]),
                       r=[("qk", sl, i) for i in range(base + lo, base + lo + n)], w=[("st" + nm, ci)],
                       dma="qs%s%d" % (nm, sl))
            for s in range(4):
                bi = cnt["pf"] % 6; cnt["pf"] += 1
                ps = pf[bi]
                for cc in range(8):
                    sch.op("pe", lambda e, cc=cc, ps=ps, s=s: e.matmul(
                        ps[:, 0:256], lhsT=hT[:, cc, s * 128:(s + 1) * 128], rhs=win[:, cc, 2816:3072],
                        start=(cc == 0), stop=(cc == 7)),
                           r=[("win", 11), ("hT",)], w=[("pf", bi)])
                src = ps[:, 0:256].rearrange("p (h e) -> p h e", e=64)
                dst = vt[:, sl, :, s, 0:64]
                sch.op("dve", lambda e, src=src, dst=dst: e.tensor_copy(out=dst, in_=src),
                       r=[("pf", bi)], w=[("vtw", sl, s)])
            sch.op("sp", lambda e, sl=sl, ci=ci: e.dma_start(
                out=Vv[:, :, ci * 4:(ci + 1) * 4, :], in_=vt[:, sl, :, :, :]),
                   r=[("vt", sl)] + [("vtw", sl, s) for s in range(4)],
                   w=[("V", ci)], dma="vs%d" % sl)

        phaseA_norm(0)
        for j in range(12):
            _load_weight_cols(sch, win, C.w_in, j * 256, (j + 1) * 256, ("win", j), j)
        phaseA_tr(0)
        for ci in range(8):
            if ci + 1 < 8:
                phaseA_norm(ci + 1)
            phaseB(ci)
            if ci + 1 < 8:
                phaseA_tr(ci + 1)
        sch.emit_stage()


def attn_stage(nc, sch, C, heads=range(16)):
    with ExitStack() as es:
        T = lambda n, shp, dt: es.enter_context(nc.sbuf_tensor("at" + n, shp, dt))
        qt = T("qt", [128, 2, S], BF16)
        kt = T("kt", [128, 2, S], BF16)
        vtT = T("vtT", [128, 2, S], BF16)
        vt = T("vt", [128, 2, 32, 65], BF16)
        tbs = T("tbs", [128, 2, 3, 256], F32)
        cms = T("cms", [128, 256], F32)
        Ws = T("Ws", [128, 2, 3, 256], BF16)
        vk = T("vk", [128, 3, 65], BF16)
        acc = T("acc", [128, S], F32)
        tbm = T("tbm", [128, 2, NW], F32)
        cm = T("cm", [128, NW], F32)
        Wm = T("Wm", [128, 2, NW], BF16)
        Eb = T("Eb", [128, 3, 512], BF16)
        Pb = T("Pb", [128, 3, 512], BF16)
        oh = T("oh", [128, 2, 32, 64], F32)
        km = T("km", [128, 80], BF16)
        kmf = T("kmf", [128, 16], F32)
        gmf = T("gmf", [128, 4, 16], F32)
        m8 = T("m8", [128, 4, 8], F32)
        selin = T("selin", [128, 4, 80], F32)
        mcon = T("mcon", [128, 16, 16], F32)
        rd = T("rd", [128, 8], F32)
        idf = T("idf", [128, 128], F32)
        psS = [es.enter_context(nc.psum_tensor("psS%d" % i, [128, 512], F32)) for i in range(2)]
        psO = [es.enter_context(nc.psum_tensor("psO%d" % i, [128, 512], F32)) for i in range(4)]
        psM = es.enter_context(nc.psum_tensor("psM", [128, 512], F32))
        psVb = es.enter_context(nc.psum_tensor("psVb", [128, 1024], BF16))
        psG = psM

        sch.op("sp", lambda e: e.dma_start(out=idf[:, :], in_=C.identf), w=[("idf",)], dma="c2")
        sch.op("sp", lambda e: e.dma_start(out=cm[:, :], in_=C.cmask), w=[("cm",)], dma="c0")
        sch.op("sp", lambda e: e.dma_start(out=cms[:, :], in_=C.cms), w=[("cms",)], dma="c3")
        sch.op("sp", lambda e: e.dma_start(out=mcon[:, :, :], in_=C.mconst), w=[("mcon",)], dma="c1")
        for sl in range(2):
            sch.op("pool", lambda e, sl=sl: e.memset(kt[64:128, sl, :], 0.0), w=[("ktaug", sl)])
            sch.op("pool", lambda e, sl=sl: e.memset(qt[64:128, sl, :], 0.0), w=[("qtz", sl)])
            sch.op("pool", lambda e, sl=sl: e.memset(vtT[64:128, sl, :], 0.0), w=[("vtz", sl)])
            sch.op("pool", lambda e, sl=sl: e.dma_start(
                out=kt[64:80, sl, :].rearrange("j (a b) -> j a b", b=2048),
                in_=C.kbind.rearrange("j (a b) -> j a b", b=2048)),
                   w=[("ktaug", sl)], dma="w%d" % sl)
        sch.op("pool", lambda e: e.memset(km[:, :], 0.0), w=[("km",)])
        sch.op("pool", lambda e: e.memset(selin[:, :, :], 0.0), w=[("selin", 0), ("selin", 1), ("selin", 2), ("selin", 3)])
        sch.op("pool", lambda e: e.memset(vk[:, :, 64:65], 1.0), w=[("vk", 0), ("vk", 1), ("vk", 2)])
        Ov = C.O.rearrange("t p f -> p t f")

        def load_head(h):
            sl = h % 2
            sch.op("sp", lambda e: e.dma_start(out=qt[0:64, sl, :], in_=C.qT[h * 64:(h + 1) * 64, :]),
                   w=[("qt", sl)], dma="hq%d" % sl)
            sch.op("sp", lambda e: e.dma_start(out=kt[0:64, sl, :], in_=C.kT[h * 64:(h + 1) * 64, :]),
                   w=[("kt", sl)], dma="hk%d" % sl)
            if h < 12:
                sch.op("sp", lambda e: e.dma_start(out=vtT[0:64, sl, :], in_=C.vT[h * 64:(h + 1) * 64, :]),
                       w=[("vtT", sl)], dma="hv%d" % sl)
                sch.op("sp", lambda e: e.dma_start(out=tbs[:, sl, :, :], in_=C.tbs[h]),
                       w=[("tbs", sl)], dma="ht%d" % sl)
            else:
                sch.op("sp", lambda e: e.dma_start(out=vt[:, sl, :, :], in_=C.V[h - 12]),
                       w=[("vth", sl)], dma="hv%d" % sl)
                sch.op("sp", lambda e: e.dma_start(out=tbm[:, sl, :], in_=C.tb[h - 12]),
                       w=[("tbm", sl)], dma="ht%d" % sl)

        def dilated_head(h, sl):
            sch.op("act", lambda e: e.activation(out=tbs[:, sl, :, :], in_=tbs[:, sl, :, :], func=AF.Exp),
                   r=[("tbs", sl)], w=[("tbs", sl)])
            for di in range(3):
                sch.op("dve", lambda e, di=di: e.tensor_tensor(out=Ws[:, sl, di, :], in0=tbs[:, sl, di, :],
                                                             in1=cms[:, :], op=ALU.mult),
                       r=[("tbs", sl), ("cms",)], w=[("Ws", sl)])
            items = []
            for di, d in enumerate((1, 4, 16)):
                nt_ = (S // d) // 128
                for r in range(d):
                    for jt in range(nt_):
                        items.append((di, d, r, jt, nt_))
            n = len(items)

            psS3 = [psS[0], psS[1], psO[3]]
            psS3tok = [("psS", 0), ("psS", 1), ("psO", 3)]

            def front(i):
                di, d, r, jt, nt_ = items[i]
                b = i % 3
                b3 = i % 3
                nq = 256 if jt + 1 < nt_ else 128
                ks = jt * 128 * d + r
                lhs = kt[0:128, sl, ks:ks + 127 * d + 1:d]
                rhs = qt[0:128, sl, ks:ks + (nq - 1) * d + 1:d]
                pS = psS3[b]
                sch.op("pe", lambda e, lhs=lhs, rhs=rhs, pS=pS, nq=nq: e.matmul(
                    pS[:, 0:nq], lhsT=lhs, rhs=rhs, start=True, stop=True),
                       r=[("kt", sl), ("ktaug", sl), ("qt", sl), ("qtz", sl)], w=[psS3tok[b]])
                eb = Eb[:, b3, 0:nq]
                sch.op("act", lambda e, eb=eb, pS=pS, nq=nq: e.activation(out=eb, in_=pS[:, 0:nq], func=AF.Exp,
                                                                       scale=0.125),
                       r=[psS3tok[b]], w=[("Eb", b3)])
                pbv = Pb[:, b3, 0:nq]
                wv = Ws[:, sl, di, 0:nq]
                sch.op("pool" if i % 2 == 1 else "dve",
                       lambda e, pbv=pbv, eb=eb, wv=wv: e.tensor_tensor(out=pbv, in0=eb, in1=wv, op=ALU.mult),
                       r=[("Eb", b3), ("Ws", sl)], w=[("Pb", b3)])
                vin = vtT[0:128, sl, ks:ks + 127 * d + 1:d]
                sch.op("pe", lambda e, vin=vin: e.transpose(out=psVb[:, 0:128], in_=vin, identity=C.ident[:, :]),
                       r=[("vtT", sl), ("vtz", sl), ("ident",)], w=[("psVb",)])
                vkv = vk[:, b3, 0:64]
                sch.op("act", lambda e, vkv=vkv: e.copy(out=vkv, in_=psVb[:, 0:64]),
                       r=[("psVb",)], w=[("vk", b3)])

            def back(i):
                di, d, r, jt, nt_ = items[i]
                b3 = i % 3
                for qi in range(2):
                    jq = jt + qi
                    if jq >= nt_:
                        continue
                    bank = jq % 2
                    po = psO[bank][0:65, 0:128]
                    lhs = vk[:, b3, 0:65]
                    rhs = Pb[:, b3, qi * 128:(qi + 1) * 128]
                    st_ = (qi == 1) or (jq == 0)
                    sp_ = (qi == 0)
                    sch.op("pe", lambda e, po=po, lhs=lhs, rhs=rhs, st_=st_, sp_=sp_: e.matmul(
                        po, lhsT=lhs, rhs=rhs, start=st_, stop=sp_),
                           r=[("vk", b3), ("Pb", b3)], w=[("psO", bank)])
                    if sp_:
                        qs = jq * 128 * d + r
                        av = acc[0:65, qs:qs + 127 * d + 1:d]
                        if di == 0:
                            sch.op("act", lambda e, av=av, po=po: e.copy(out=av, in_=po),
                                   r=[("psO", bank)], w=[("acc",)])
                        else:
                            sch.op("dve", lambda e, av=av, po=po: e.tensor_tensor(out=av, in0=av, in1=po, op=ALU.add),
                                   r=[("psO", bank), ("acc",)], w=[("acc",)])

            for i in range(n + 2):
                if i < n:
                    front(i)
                if i >= 2:
                    back(i - 2)
            for t in range(NT):
                s4 = t % 4
                pF = psO[2 + t % 2]
                sch.op("pe", lambda e, t=t, pF=pF: e.transpose(out=pF[:, 0:65], in_=acc[0:65, t * 128:(t + 1) * 128],
                                                           identity=idf[0:65, 0:65]),
                       r=[("acc",), ("idf",)], w=[("psO", 2 + t % 2)])
                sch.op("dve", lambda e, s4=s4, pF=pF: e.reciprocal(out=rd[:, s4:s4 + 1], in_=pF[:, 64:65]),
                       r=[("psO", 2 + t % 2)], w=[("rd", s4)])
                sch.op("dve", lambda e, s4=s4, pF=pF, t=t: e.tensor_scalar(
                    out=oh[:, sl, t, :], in0=pF[:, 0:64], scalar1=rd[:, s4:s4 + 1], scalar2=None, op0=ALU.mult),
                       r=[("psO", 2 + t % 2), ("rd", s4)], w=[("oh", sl, t)])
                if s4 == 3:
                    qc = t // 4
                    sch.op("sp", lambda e, qc=qc: e.dma_start(
                        out=Ov[:, qc * 4:(qc + 1) * 4, h * 64:(h + 1) * 64], in_=oh[:, sl, qc * 4:(qc + 1) * 4, :]),
                           r=[("oh", sl, qc * 4 + s) for s in range(4)], w=[("O", h, qc)], dma="os%d" % (qc % 4))

        def moba_head(h, sl):
            K = 128
            sch.op("act", lambda e: e.activation(out=tbm[:, sl, :], in_=tbm[:, sl, :], func=AF.Exp),
                   r=[("tbm", sl)], w=[("tbm", sl)])
            sch.op("dve", lambda e: e.tensor_tensor(out=Wm[:, sl, :], in0=tbm[:, sl, :], in1=cm[:, :], op=ALU.mult),
                   r=[("tbm", sl), ("cm",)], w=[("Wm", sl)])
            ktok = [("kt", sl), ("ktaug", sl)]
            sch.op("dve", lambda e: e.tensor_reduce(
                out=kmf[0:64, :], in_=kt[0:64, sl, :].rearrange("p (a b) -> p a b", b=256), axis=AX.X, op=ALU.add),
                   r=[("kt", sl)], w=[("kmf",)])
            sch.op("dve", lambda e: e.tensor_copy(out=km[0:64, 64:80], in_=kmf[0:64, :]),
                   r=[("kmf",)], w=[("km",)])
            gbanks = [psS[0], psS[1], psO[0], psO[1], psO[2], psO[3]]
            gtoks = [("psS", 0), ("psS", 1), ("psO", 0), ("psO", 1), ("psO", 2), ("psO", 3)]

            def gate_front(t):
                n = t // 2
                g = t % 4
                bk, bt = gbanks[t % 6], gtoks[t % 6]
                sch.op("pe", lambda e: e.matmul(
                    bk[:, 0:80], lhsT=qt[0:128, sl, t * 128:(t + 1) * 128], rhs=km[0:128, 0:80],
                    start=True, stop=True), r=[("qt", sl), ("qtz", sl), ("km",)], w=[bt])
                sch.op("dve", lambda e: e.tensor_tensor(
                    out=gmf[:, g, :], in0=bk[:, 64:80], in1=mcon[:, n, :], op=ALU.add),
                       r=[bt, ("mcon",)], w=[("gmf", g)])
                sch.op("dve", lambda e: e.max(out=m8[:, g, :], in_=gmf[:, g, :]),
                       r=[("gmf", g)], w=[("m8", g)])
                sch.op("dve", lambda e: e.tensor_scalar(
                    out=selin[:, g, 64:80], in0=gmf[:, g, :], scalar1=m8[:, g, 3:4], scalar2=1.0,
                    op0=ALU.is_ge, op1=ALU.subtract), r=[("gmf", g), ("m8", g)], w=[("selin", g)])

            def gate_back(t):
                g = t % 4
                bk, bt = gbanks[t % 6], gtoks[t % 6]
                sch.op("pe", lambda e: e.transpose(out=bk[0:80, 128:256], in_=selin[:, g, 0:80],
                                                   identity=idf[:, :]),
                       r=[("selin", g), ("idf",)], w=[bt])
                sch.op("act", lambda e: e.copy(out=qt[64:80, sl, t * 128:(t + 1) * 128],
                                               in_=bk[64:80, 128:256]),
                       r=[bt], w=[("qtaug", sl, t)])

            for t in range(NT + 2):
                if t < NT:
                    gate_front(t)
                if t >= 2:
                    gate_back(t - 2)
            for qc in range(8):
                ktl = list(range(0, qc * 4 + 4))
                nk = len(ktl)

                def valid(kti, s, qc=qc):
                    return kti * 128 <= qc * 512 + s * 128 + 127

                firsts = {s: min(k for k in ktl if valid(k, s)) for s in range(4)}
                lasts = {s: max(k for k in ktl if valid(k, s)) for s in range(4)}
                qr = [("qt", sl), ("qtz", sl)] + [("qtaug", sl, qc * 4 + s) for s in range(4)]

                mS3 = [psS[0], psS[1], psM]
                mS3tok = [("psS", 0), ("psS", 1), ("psM",)]

                def QK(i, qc=qc, ktl=ktl, qr=qr):
                    kti = ktl[i]
                    b3 = i % 3
                    lhs = kt[0:K, sl, kti * 128:(kti + 1) * 128]
                    rhs = qt[0:K, sl, qc * 512:(qc + 1) * 512]
                    pS = mS3[b3]
                    sch.op("pe", lambda e, lhs=lhs, rhs=rhs, pS=pS: e.matmul(
                        pS[:, :], lhsT=lhs, rhs=rhs, start=True, stop=True),
                           r=ktok + qr, w=[mS3tok[b3]])
                    eb = Eb[:, b3, :]
                    sch.op("act", lambda e, eb=eb, pS=pS: e.activation(out=eb, in_=pS[:, :], func=AF.Exp, scale=0.125),
                           r=[mS3tok[b3]], w=[("Eb", b3)])
                    off = min(qc * 512 - kti * 128, 2048) + WOFF
                    pbv = Pb[:, b3, :]
                    wv = Wm[:, sl, off:off + 512]
                    sch.op("pool" if i % 3 == 1 else "dve",
                           lambda e, pbv=pbv, eb=eb, wv=wv: e.tensor_tensor(out=pbv, in0=eb, in1=wv, op=ALU.mult),
                           r=[("Eb", b3), ("Wm", sl)], w=[("Pb", b3)])

                def PV(i, ktl=ktl, firsts=firsts, lasts=lasts, valid=valid):
                    kti = ktl[i]
                    b3 = i % 3
                    for s in range(4):
                        if not valid(kti, s):
                            continue
                        po = psO[s][:, 0:65]
                        lhs = Pb[:, b3, s * 128:(s + 1) * 128]
                        rhs = vt[:, sl, kti, :]
                        st_ = (kti == firsts[s])
                        sp_ = (kti == lasts[s])
                        sch.op("pe", lambda e, po=po, lhs=lhs, rhs=rhs, st_=st_, sp_=sp_: e.matmul(
                            po, lhsT=lhs, rhs=rhs, start=st_, stop=sp_),
                               r=[("Pb", b3), ("vth", sl)], w=[("psO", s)])

                for i in range(nk + 2):
                    if i < nk:
                        QK(i)
                    if i >= 2:
                        PV(i - 2)
                for s in range(4):
                    t = qc * 4 + s
                    sch.op("dve", lambda e, s=s: e.reciprocal(out=rd[:, 4 + s:5 + s], in_=psO[s][:, 64:65]),
                           r=[("psO", s)], w=[("rd", 4 + s)])
                    sch.op("dve", lambda e, s=s, t=t: e.tensor_scalar(
                        out=oh[:, sl, t, :], in0=psO[s][:, 0:64], scalar1=rd[:, 4 + s:5 + s], scalar2=None,
                        op0=ALU.mult), r=[("psO", s), ("rd", 4 + s)], w=[("oh", sl, t)])
                sch.op("sp", lambda e, qc=qc: e.dma_start(
                    out=Ov[:, qc * 4:(qc + 1) * 4, h * 64:(h + 1) * 64], in_=oh[:, sl, qc * 4:(qc + 1) * 4, :]),
                       r=[("oh", sl, qc * 4 + s) for s in range(4)], w=[("O", h, qc)], dma="os%d" % (qc % 4))

        hl = list(heads)
        load_head(hl[0])
        for hi, h in enumerate(hl):
            sl = h % 2
            if hi + 1 < len(hl):
                load_head(hl[hi + 1])
            if h < 12:
                dilated_head(h, sl)
            else:
                moba_head(h, sl)
        sch.emit_stage()


def cross_stage(nc, sch, C):
    with ExitStack() as es0:
        T0 = lambda n, shp, dt: es0.enter_context(nc.sbuf_tensor("cr" + n, shp, dt))
        kcT = T0("kcT", [128, 8, 256], BF16)
        vc = T0("vc", [128, 2, D], BF16)
        with ExitStack() as es:
            T = lambda n, shp, dt: es.enter_context(nc.sbuf_tensor("cm" + n, shp, dt))
            wckv = T("wckv", [128, 8, 2 * D], BF16)
            gM = T("gM", [128, D], F32)
            mt_ = T("mt", [128, 2, D], F32)
            hn = T("hn", [128, 2, D], BF16)
            hT = T("hT", [128, 8, 256], BF16)
            pf, pb = _psum_banks(nc, es, 6, 2)
            _load_gain(sch, C, gM, 4, "M")
            for mt in range(2):
                sch.op("sp", lambda e, mt=mt: e.dma_start(out=mt_[:, mt, :], in_=C.mem[mt * 128:(mt + 1) * 128, :]),
                       w=[("mt", mt)], dma="ol%d" % mt)
                _norm_to_hn(sch, C, mt_[:, mt, :], ("mt", mt), gM, "M", hn[:, mt, :], ("hn", mt), mt)
            for j in range(8):
                _load_weight_cols(sch, wckv, C.w_ckv, j * 256, (j + 1) * 256, ("wckv", j), j)
            for mt in range(2):
                _transpose_to(sch, C, lambda cc, mt=mt: hn[:, mt, cc * 128:(cc + 1) * 128], ("hn", mt),
                              pb, mt, hT, ("hT",), mt * 128)
            nb = [0]

            def nextpf0():
                bi = nb[0] % 6
                nb[0] += 1
                return bi
            for fc in range(8):
                bi = nextpf0()
                for cc in range(8):
                    sch.op("pe", lambda e, cc=cc, fc=fc, bi=bi: e.matmul(
                        pf[bi][:, 0:256], lhsT=wckv[:, cc, fc * 128:(fc + 1) * 128], rhs=hT[:, cc, 0:256],
                        start=(cc == 0), stop=(cc == 7)), r=[("wckv", fc // 2), ("hT",)], w=[("pf", bi)])
                sch.op("dve", lambda e, fc=fc, bi=bi: e.tensor_copy(out=kcT[:, fc, :], in_=pf[bi][:, 0:256]),
                       r=[("pf", bi)], w=[("kcT",)])
            for mt in range(2):
                for half in range(2):
                    bi = nextpf0()
                    for cc in range(8):
                        sch.op("pe", lambda e, cc=cc, mt=mt, half=half, bi=bi: e.matmul(
                            pf[bi][:, :], lhsT=hT[:, cc, mt * 128:(mt + 1) * 128],
                            rhs=wckv[:, cc, D + half * 512:D + (half + 1) * 512],
                            start=(cc == 0), stop=(cc == 7)),
                               r=[("wckv", 4 + half * 2), ("wckv", 5 + half * 2), ("hT",)], w=[("pf", bi)])
                    sch.op("dve", lambda e, mt=mt, half=half, bi=bi: e.tensor_copy(
                        out=vc[:, mt, half * 512:(half + 1) * 512], in_=pf[bi][:, :]),
                           r=[("pf", bi)], w=[("vc",)])
            sch.emit_stage()

        with ExitStack() as es:
            T = lambda n, shp, dt: es.enter_context(nc.sbuf_tensor("cr" + n, shp, dt))
            wout = T("wout", [128, 8, D], BF16)
            wcq = T("wcq", [128, 8, D], BF16)
            wco = T("wco", [128, 8, D], BF16)
            gO = T("gO", [128, D], F32)
            gC = T("gC", [128, D], F32)
            ot = T("ot", [128, 2, D], F32)
            x1t = T("x1t", [128, 4, D], F32)
            x2 = T("x2", [128, 2, 4, D], F32)
            hn = T("hn", [128, 4, D], BF16)
            yT = T("yT", [128, 8, 512], BF16)
            hT = T("hT", [128, 2, 8, 512], BF16)
            qcT = T("qcT", [128, 8, 512], BF16)
            Pc = T("Pc", [128, 2, 2, 512], BF16)
            rdt = T("rdt", [128, 2, 512], F32)
            ocT = T("ocT", [128, 8, 512], BF16)
            pf, pb = _psum_banks(nc, es, 6, 2)
            cnt = {"pf": 0, "k": 0}

            def nextpf():
                bi = cnt["pf"] % 6
                cnt["pf"] += 1
                return bi

            _load_gain(sch, C, gO, 2, "O")
            _load_gain(sch, C, gC, 3, "C")

            def load_w():
                ki = 0
                for (wt, wd, nm) in ((wout, C.w_out, "wout"), (wcq, C.w_cq, "wcq"), (wco, C.w_co, "wco")):
                    for j in range(4):
                        _load_weight_cols(sch, wt, wd, j * 256, (j + 1) * 256, (nm, j), ki); ki += 1

            def P1_norm(ci):
                ks = []
                for s in range(4):
                    t = ci * 4 + s
                    sl = t % 2
                    sch.op("sp", lambda e, sl=sl, t=t: e.dma_start(out=ot[:, sl, :], in_=C.O[t]),
                           w=[("ot", sl)], dma="ol%d" % sl)
                    sch.op("sp", lambda e, s=s, t=t: e.dma_start(out=x1t[:, s, :],
                                                                in_=C.x1[t * 128:(t + 1) * 128, :]),
                           w=[("x1t", s)], dma="xl%d" % s)
                    k = cnt["k"]; cnt["k"] += 1
                    ks.append(k)
                    b = (k % 4) * 3
                    st = C.st
                    sa, sb, sc = st[:, b:b + 1], st[:, b + 1:b + 2], st[:, b + 2:b + 3]
                    b2 = 12 + (k % 4) * 3
                    sa2, sb2, sc2 = st[:, b2:b2 + 1], st[:, b2 + 1:b2 + 2], st[:, b2 + 2:b2 + 3]
                    tk = ("st", k % 4)
                    kk = k % 4
                    sch.op("act", lambda e, sl=sl, kk=kk, sa=sa: e.activation(
                        out=hn[:, kk, 0:768], in_=ot[:, sl, 0:768], func=AF.Square, accum_out=sa),
                           r=[("ot", sl)], w=[("hn", kk), tk])
                    sch.op("act", lambda e, sl=sl, kk=kk, sa2=sa2: e.activation(
                        out=hn[:, kk, 768:1024], in_=ot[:, sl, 768:1024], func=AF.Square, accum_out=sa2),
                           r=[("ot", sl)], w=[("hn", kk), tk])
                    sch.op("dve", lambda e, sa=sa, sb=sb: e.tensor_scalar(
                        out=sb, in0=sa, scalar1=1.0 / 768, scalar2=EPS, op0=ALU.mult, op1=ALU.add), r=[tk], w=[tk])
                    sch.op("dve", lambda e, sa2=sa2, sb2=sb2: e.tensor_scalar(
                        out=sb2, in0=sa2, scalar1=1.0 / 256, scalar2=EPS, op0=ALU.mult, op1=ALU.add), r=[tk], w=[tk])
                    sch.op("pool", lambda e, sb=sb, sc=sc: e.tensor_tensor(
                        out=sc, in0=sb, in1=C.mhalf[:, 0:1], op=ALU.pow), r=[tk, ("mhalf",)], w=[tk])
                    sch.op("pool", lambda e, sb2=sb2, sc2=sc2: e.tensor_tensor(
                        out=sc2, in0=sb2, in1=C.mhalf[:, 0:1], op=ALU.pow), r=[tk, ("mhalf",)], w=[tk])
                    sch.op("dve", lambda e, sl=sl, kk=kk, sc=sc: e.scalar_tensor_tensor(
                        out=hn[:, kk, 0:768], in0=ot[:, sl, 0:768], scalar=sc, in1=gO[:, 0:768],
                        op0=ALU.mult, op1=ALU.mult), r=[("ot", sl), tk, ("g", "O")], w=[("hn", kk)])
                    sch.op("dve", lambda e, sl=sl, kk=kk, sc2=sc2: e.scalar_tensor_tensor(
                        out=hn[:, kk, 768:1024], in0=ot[:, sl, 768:1024], scalar=sc2, in1=gO[:, 768:1024],
                        op0=ALU.mult, op1=ALU.mult), r=[("ot", sl), tk, ("g", "O")], w=[("hn", kk)])
                    _transpose_to(sch, C, lambda cc, kk=kk: hn[:, kk, cc * 128:(cc + 1) * 128], ("hn", kk),
                                  pb, k % 2, yT, ("yT", s), s * 128)

            def P1_wout(ci, ss=(0, 1, 2, 3)):
                xs = ci % 2
                for s in ss:
                    for half in range(2):
                        bi = nextpf()
                        for cc in range(8):
                            sch.op("pe", lambda e, cc=cc, s=s, half=half, bi=bi: e.matmul(
                                pf[bi][:, :], lhsT=yT[:, cc, s * 128:(s + 1) * 128],
                                rhs=wout[:, cc, half * 512:(half + 1) * 512], start=(cc == 0), stop=(cc == 7)),
                                   r=[("yT", s), ("wout", half * 2), ("wout", half * 2 + 1)], w=[("pf", bi)])
                        sch.op("dve", lambda e, s=s, xs=xs, half=half, bi=bi: e.tensor_tensor(
                            out=x2[:, xs, s, half * 512:(half + 1) * 512], in0=pf[bi][:, :],
                            in1=x1t[:, s, half * 512:(half + 1) * 512], op=ALU.add),
                               r=[("pf", bi), ("x1t", s)], w=[("x2", xs, s)])

            def P1_norm2(ci):
                xs = ci % 2
                for s in range(4):
                    k = cnt["k"]; cnt["k"] += 1
                    kk = k % 4
                    _norm_to_hn(sch, C, x2[:, xs, s, :], ("x2", xs, s), gC, "C", hn[:, kk, :], ("hn", kk), k)
                    _transpose_to(sch, C, lambda cc, kk=kk: hn[:, kk, cc * 128:(cc + 1) * 128], ("hn", kk),
                                  pb, k % 2, hT[:, xs], ("hT", xs), s * 128)

            def P2(ci):
                xs = ci % 2
                for fc in range(8):
                    bi = nextpf()
                    for cc in range(8):
                        sch.op("pe", lambda e, cc=cc, fc=fc, bi=bi, xs=xs: e.matmul(
                            pf[bi][:, :], lhsT=wcq[:, cc, fc * 128:(fc + 1) * 128], rhs=hT[:, xs, cc, :],
                            start=(cc == 0), stop=(cc == 7)), r=[("wcq", fc // 2), ("hT", xs)], w=[("pf", bi)])
                    sch.op("act", lambda e, fc=fc, bi=bi: e.copy(out=qcT[:, fc, :], in_=pf[bi][:, :]),
                           r=[("pf", bi)], w=[("qcT", fc)])

            def P3(ci, hds=(0, 1, 2, 3)):
                for hd in hds:
                    pslot = hd % 2
                    for mt in range(2):
                        bi = nextpf()
                        for e2 in range(2):
                            sch.op("pe", lambda e, e2=e2, mt=mt, hd=hd, bi=bi: e.matmul(
                                pf[bi][:, :], lhsT=kcT[:, 2 * hd + e2, mt * 128:(mt + 1) * 128],
                                rhs=qcT[:, 2 * hd + e2, :], start=(e2 == 0), stop=(e2 == 1)),
                                   r=[("kcT",), ("qcT", 2 * hd + e2)], w=[("pf", bi)])
                        sch.op("act", lambda e, mt=mt, pslot=pslot, bi=bi: e.activation(
                            out=Pc[:, pslot, mt, :], in_=pf[bi][:, :], func=AF.Exp, scale=1.0 / 16.0),
                               r=[("pf", bi)], w=[("Pc", pslot, mt)])
                    bd = nextpf()
                    for mt in range(2):
                        sch.op("pe", lambda e, mt=mt, pslot=pslot, bd=bd: e.matmul(
                            pf[bd][:, :], lhsT=C.ones[:, :], rhs=Pc[:, pslot, mt, :],
                            start=(mt == 0), stop=(mt == 1)),
                               r=[("ones",), ("Pc", pslot, mt)], w=[("pf", bd)])
                    sch.op("dve", lambda e, pslot=pslot, bd=bd: e.reciprocal(out=rdt[:, pslot, :], in_=pf[bd][:, :]),
                           r=[("pf", bd)], w=[("rdt", pslot)])
                    for e2 in range(2):
                        bi = nextpf()
                        for mt in range(2):
                            sch.op("pe", lambda e, e2=e2, mt=mt, hd=hd, pslot=pslot, bi=bi: e.matmul(
                                pf[bi][:, :], lhsT=vc[:, mt, (2 * hd + e2) * 128:(2 * hd + e2 + 1) * 128],
                                rhs=Pc[:, pslot, mt, :], start=(mt == 0), stop=(mt == 1)),
                                   r=[("vc",), ("Pc", pslot, mt)], w=[("pf", bi)])
                        sch.op("dve", lambda e, e2=e2, hd=hd, pslot=pslot, bi=bi: e.tensor_tensor(
                            out=ocT[:, 2 * hd + e2, :], in0=pf[bi][:, :], in1=rdt[:, pslot, :], op=ALU.mult),
                               r=[("pf", bi), ("rdt", pslot)], w=[("ocT", 2 * hd + e2)])

            def P4(ci):
                xs = ci % 2
                for s in range(4):
                    t = ci * 4 + s
                    for half in range(2):
                        bi = nextpf()
                        for cc in range(8):
                            sch.op("pe", lambda e, cc=cc, s=s, half=half, bi=bi: e.matmul(
                                pf[bi][:, :], lhsT=ocT[:, cc, s * 128:(s + 1) * 128],
                                rhs=wco[:, cc, half * 512:(half + 1) * 512], start=(cc == 0), stop=(cc == 7)),
                                   r=[("ocT", cc), ("wco", half * 2), ("wco", half * 2 + 1)], w=[("pf", bi)])
                        sch.op("dve", lambda e, s=s, xs=xs, half=half, bi=bi: e.tensor_tensor(
                            out=x2[:, xs, s, half * 512:(half + 1) * 512], in0=pf[bi][:, :],
                            in1=x2[:, xs, s, half * 512:(half + 1) * 512], op=ALU.add),
                               r=[("pf", bi), ("x2", xs, s)], w=[("x2", xs, s)])
                    sch.op("sp", lambda e, s=s, xs=xs, t=t: e.dma_start(out=C.x3[t * 128:(t + 1) * 128, :],
                                                                       in_=x2[:, xs, s, :]),
                           r=[("x2", xs, s)], w=[("x3", t)], dma="xs%d%d" % (xs, s))

            P1_norm(0)
            load_w()
            P1_wout(0)
            P1_norm2(0)
            for ci in range(8):
                P2(ci)
                if ci + 1 < 8:
                    P1_norm(ci + 1)
                for hd in range(4):
                    P3(ci, (hd,))
                    if ci + 1 < 8:
                        P1_wout(ci + 1, (hd,))
                if ci + 1 < 8:
                    P1_norm2(ci + 1)
                P4(ci)
            sch.emit_stage()


def build_program(debug=False, upto=5):
    nc = bass.Bass("TRN2", target_bir_lowering=False)
    C = Ctx()

    def din(name, shape, dt=F32):
        return nc.dram_tensor(name, shape, dt, kind="ExternalInput")

    def dscr(name, shape, dt):
        return nc.dram_tensor(name, shape, dt, kind=("ExternalOutput" if debug else "Internal"))

    C.x = din("x", [S, D]).ap()
    C.mem = din("mem", [256, D]).ap()
    C.w_ffn1_gu = din("w_ffn1_gu", [D, 2 * DFF]).ap()
    C.w_ffn1_down = din("w_ffn1_down", [DFF, D]).ap()
    C.w_in = din("w_in", [D, 3 * D]).ap()
    C.w_out = din("w_out", [D, D]).ap()
    C.w_cq = din("w_cq", [D, D]).ap()
    C.w_ckv = din("w_ckv", [D, 2 * D]).ap()
    C.w_co = din("w_co", [D, D]).ap()
    C.w_ffn2_gu = din("w_ffn2_gu", [D, 2 * DFF]).ap()
    C.w_ffn2_down = din("w_ffn2_down", [DFF, D]).ap()
    C.gall_t = din("gall", [7, D])
    C.tb = din("tb", [4, 128, NW]).ap()
    C.cmask = din("cmask", [128, NW]).ap()
    C.tbs = din("tbs", [12, 128, 3, 256]).ap()
    C.cms = din("cms", [128, 256]).ap()
    C.kbind = din("kbind", [16, S]).ap()
    C.mconst = din("mconst", [128, 16, 16]).ap()
    C.identf = din("identf", [128, 128]).ap()
    C.out = nc.dram_tensor("out", [S, D], F32, kind="ExternalOutput").ap()
    C.x1 = dscr("x1", [S, D], F32).ap()
    C.qT = dscr("qT", [D, S], BF16).ap()
    C.kT = dscr("kT", [D, S], BF16).ap()
    C.vT = dscr("vT", [768, S], BF16).ap()
    C.V = dscr("V", [4, 128, NT, 65], BF16).ap()
    C.O = dscr("O", [NT, 128, D], F32).ap()
    C.x3 = dscr("x3", [S, D], F32).ap()

    with ExitStack() as es:
        C.ident = es.enter_context(nc.sbuf_tensor("ident", [128, 128], BF16))
        C.ones = es.enter_context(nc.sbuf_tensor("ones", [128, 128], BF16))
        C.mhalf = es.enter_context(nc.sbuf_tensor("mhalf", [128, 1], F32))
        C.st = es.enter_context(nc.sbuf_tensor("st", [128, 24], F32))
        sch = Sched(nc, es)
        sch.op("pool", lambda e: e.dma_start(out=C.ident[:, :], in_=C.identf), w=[("ident",)], dma="w0")
        sch.op("pool", lambda e: e.memset(C.ones[:, :], 1.0), w=[("ones",)])
        sch.op("pool", lambda e: e.memset(C.mhalf[:, :], -0.5), w=[("mhalf",)])
        sch.emit_stage()

        if upto >= 1:
            ffn_stage(nc, sch, C, "f1", C.x, C.x1, C.w_ffn1_gu, C.w_ffn1_down, 0, None)
        if upto >= 2:
            inproj_stage(nc, sch, C)
        if upto >= 3:
            attn_stage(nc, sch, C)
        if upto >= 4:
            cross_stage(nc, sch, C)
        if upto >= 5:
            ffn_stage(nc, sch, C, "f2", C.x3, C.out, C.w_ffn2_gu, C.w_ffn2_down, 5, 6)
        sch.final_wait()
    return nc


def _rel_bucket_np(dist):
    n = np.maximum(dist, 0)
    nf = np.maximum(n, 1).astype(np.float32)
    large = 16 + (np.log(nf / np.float32(16.0)) / np.float32(math.log(2048 / 16)) * np.float32(16)).astype(np.int32)
    large = np.minimum(large, 31)
    return np.where(n < 16, n, large)


def _host_consts(rel_bias):
    p = np.arange(128)[:, None]
    j = np.arange(NW)[None, :]
    Dm = j - WOFF - p
    bk = _rel_bucket_np(Dm)
    tb = np.ascontiguousarray(np.transpose(rel_bias[bk, 12:16], (2, 0, 1))).astype(np.float32)
    cmask = (Dm >= 0).astype(np.float32)
    c = np.arange(256)[None, :]
    dsub = np.maximum(c - p, 0)
    tbs = np.zeros((12, 128, 3, 256), np.float32)
    for di, d in enumerate((1, 4, 16)):
        bks = _rel_bucket_np(dsub * d)
        tbs[:, :, di, :] = np.transpose(rel_bias[bks, 0:12], (2, 0, 1))
    cms = (((c - p) >= 0) & ((c - p) <= 128)).astype(np.float32)
    kbind = np.zeros((16, S), np.float32)
    for jb in range(16):
        kbind[jb, jb * 256:(jb + 1) * 256] = 30000.0
    mconst = np.zeros((128, 16, 16), np.float32)
    for n in range(16):
        mconst[:, n, n] = 1e30
        mconst[:, n, n + 1:] = -1e30
    ident = np.eye(128, dtype=np.float32)
    return tb, cmask, tbs, cms, kbind, mconst, ident


_CACHE = {}


def _get_program(debug=False, upto=5):
    key = (debug, upto)
    if key not in _CACHE:
        _CACHE[key] = build_program(debug, upto)
    return _CACHE[key]


def _in_maps(inputs):
    f = lambda a: np.ascontiguousarray(np.asarray(a, dtype=np.float32))
    x = f(inputs["x"])
    mem = f(inputs["mem"])
    rel_bias = f(inputs["rel_bias"])
    tb, cmask, tbs, cms, kbind, mconst, ident = _host_consts(rel_bias)
    gall = np.stack([
        f(inputs["g_ffn1"])[0], f(inputs["g_mix"])[0],
        np.concatenate([f(inputs["g_out_a"])[0], f(inputs["g_out_b"])[0]]),
        f(inputs["g_cross"])[0], f(inputs["g_mem"])[0], f(inputs["g_ffn2"])[0], f(inputs["g_final"]),
    ]).astype(np.float32)
    shared = {
        "w_ffn1_gu": f(inputs["w_ffn1_gu"])[0], "w_ffn1_down": f(inputs["w_ffn1_down"])[0],
        "w_in": f(inputs["w_in"])[0], "w_out": f(inputs["w_out"])[0], "w_cq": f(inputs["w_cq"])[0],
        "w_ckv": f(inputs["w_ckv"])[0], "w_co": f(inputs["w_co"])[0],
        "w_ffn2_gu": f(inputs["w_ffn2_gu"])[0], "w_ffn2_down": f(inputs["w_ffn2_down"])[0],
        "gall": gall, "tb": tb, "cmask": cmask, "tbs": tbs, "cms": cms, "kbind": kbind, "mconst": mconst, "identf": ident,
    }
    maps = []
    for b in range(NCORES):
        m = dict(shared)
        m["x"] = np.ascontiguousarray(x[b])
        m["mem"] = np.ascontiguousarray(mem[b])
        maps.append(m)
    return maps


def kernel(**inputs):
    nc = _get_program(False, 5)
    maps = _in_maps(inputs)
    res = run_bass_kernel_spmd(nc, maps, core_ids=list(range(NCORES)))
    out = np.stack([np.asarray(r["out"], dtype=np.float32).reshape(S, D) for r in res.results], axis=0)
    return out
```

```python
import math
from contextlib import ExitStack

import numpy as np
import concourse.bass as bass
import concourse.mybir as mybir
from concourse.bass_utils import run_bass_kernel_spmd

F32 = mybir.dt.float32
BF16 = mybir.dt.bfloat16
AF = mybir.ActivationFunctionType
ALU = mybir.AluOpType
AX = mybir.AxisListType

S = 4096
D = 1024
DFF = 2816
NT = S // 128
NW = 2944
WOFF = 384
EPS = 1e-6
NCORES = 8


class Op:
    __slots__ = ("eng", "fn", "deps", "dma", "needs", "sem", "val")


class Sched:
    ENG = ("pe", "act", "dve", "pool", "sp")

    def __init__(self, nc, es):
        self.nc = nc
        self.es = es
        self.ops = {e: [] for e in self.ENG}
        self.tok = {}
        self.dma_last = {}
        self.dma_cnt = {}
        self.dma_sem = {}
        self.eng_sem = {}
        self.eng_cnt = {e: 0 for e in self.ENG}
        self.tail = {e: None for e in self.ENG}
        self.waited = {e: {} for e in self.ENG}
        self.barrier_deps = []
        for e in ("pe", "act", "dve", "pool"):
            self.eng_sem[e] = es.enter_context(nc.semaphore("sem_" + e))

    def _dsem(self, key):
        if key not in self.dma_sem:
            self.dma_sem[key] = self.es.enter_context(self.nc.semaphore("dsem_%d" % len(self.dma_sem)))
            self.dma_cnt[key] = 0
        return self.dma_sem[key]

    def op(self, eng, fn, r=(), w=(), dma=None):
        o = Op()
        o.eng, o.fn, o.dma, o.needs, o.deps = eng, fn, dma, False, []
        o.sem, o.val = None, 0
        deps = {}

        def add(d):
            if d is not None and d is not o:
                deps[id(d)] = d

        for t in r:
            st = self.tok.get(t)
            if st:
                add(st[0])
        for t in w:
            st = self.tok.get(t)
            if st:
                add(st[0])
                for rd in st[1].values():
                    add(rd)
                for rd in st[2]:
                    add(rd)
        if dma is not None:
            add(self.dma_last.get(dma))
            self.dma_last[dma] = o
            self._dsem(dma)
        for t in r:
            st = self.tok.setdefault(t, [None, {}, []])
            if dma is None:
                st[1][eng] = o
            else:
                st[2].append(o)
        for t in w:
            self.tok[t] = [o, {}, []]
        for d in deps.values():
            if d.dma is None and dma is None and d.eng == eng and eng == "pe":
                continue
            d.needs = True
            o.deps.append(d)
        self.ops[eng].append(o)
        if dma is None:
            self.tail[eng] = o
        return o

    def emit_stage(self):
        nc = self.nc
        for e in self.ENG:
            for o in self.ops[e]:
                if o.dma is not None:
                    self.dma_cnt[o.dma] += 1
                    o.sem = self.dma_sem[o.dma]
                    o.val = 16 * self.dma_cnt[o.dma]
                elif o.needs or o is self.tail[e]:
                    o.needs = True
                    self.eng_cnt[e] += 1
                    o.sem = self.eng_sem[e]
                    o.val = self.eng_cnt[e]
        bdeps = list(self.barrier_deps)

        def run(ename, eobj):
            waited = self.waited[ename]

            def wait_for(pairs):
                best = {}
                for sem, val in pairs:
                    k = id(sem)
                    if k not in best or best[k][1] < val:
                        best[k] = (sem, val)
                for k, (sem, val) in best.items():
                    if waited.get(k, 0) >= val:
                        continue
                    eobj.wait_ge(sem, val)
                    waited[k] = val

            wait_for(bdeps)
            for o in self.ops[ename]:
                wait_for([(d.sem, d.val) for d in o.deps])
                ins = o.fn(eobj)
                if o.dma is not None:
                    ins.then_inc(o.sem, 16)
                elif o.needs:
                    ins.then_inc(o.sem, 1)

        with nc.Block() as block:
            @block.tensor
            def _(e):
                run("pe", e)

            @block.scalar
            def _(e):
                run("act", e)

            @block.vector
            def _(e):
                run("dve", e)

            @block.gpsimd
            def _(e):
                run("pool", e)

            @block.sync
            def _(e):
                run("sp", e)

        nb = []
        for e in ("pe", "act", "dve", "pool"):
            if self.eng_cnt[e] > 0:
                nb.append((self.eng_sem[e], self.eng_cnt[e]))
        for k, sem in self.dma_sem.items():
            if self.dma_cnt[k] > 0:
                nb.append((sem, 16 * self.dma_cnt[k]))
        self.barrier_deps = nb
        self.ops = {e: [] for e in self.ENG}
        self.tok = {}
        self.dma_last = {}
        self.tail = {e: None for e in self.ENG}

    def final_wait(self):
        nc = self.nc
        bdeps = list(self.barrier_deps)
        with nc.Block() as block:
            @block.sync
            def _(e):
                for sem, val in bdeps:
                    e.wait_ge(sem, val)

            @block.gpsimd
            def _(e):
                for sem, val in bdeps:
                    e.wait_ge(sem, val)


class Ctx:
    pass


_PSN = [0]


def _psum_banks(nc, es, n_f32, n_bf16):
    _PSN[0] += 1
    u = _PSN[0]
    pf = [es.enter_context(nc.psum_tensor("pf%d_%d" % (u, i), [128, 512], F32)) for i in range(n_f32)]
    pb = [es.enter_context(nc.psum_tensor("pb%d_%d" % (u, i), [128, 1024], BF16)) for i in range(n_bf16)]
    return pf, pb


def _load_gain(sch, C, dst, row, key):
    src = bass.AP(C.gall_t, row * D, [[0, 128], [1, D]])
    sch.op("sp", lambda e: e.dma_start(out=dst[:, :], in_=src), w=[("g", key)], dma="g" + key)


def _norm_to_hn(sch, C, xap, xtok, gB, gkey, hn_ap, hntok, k):
    st = C.st
    b = (k % 4) * 3
    sa, sb, sc = st[:, b:b + 1], st[:, b + 1:b + 2], st[:, b + 2:b + 3]
    tk = ("st", k % 4)
    sch.op("act", lambda e: e.activation(out=hn_ap, in_=xap, func=AF.Square, accum_out=sa),
           r=[xtok], w=[hntok, tk])
    sch.op("dve", lambda e: e.tensor_scalar(out=sb, in0=sa, scalar1=1.0 / D, scalar2=EPS,
                                            op0=ALU.mult, op1=ALU.add), r=[tk], w=[tk])
    sch.op("pool", lambda e: e.tensor_tensor(out=sc, in0=sb, in1=C.mhalf[:, 0:1], op=ALU.pow),
           r=[tk, ("mhalf",)], w=[tk])
    sch.op("dve", lambda e: e.scalar_tensor_tensor(out=hn_ap, in0=xap, scalar=sc, in1=gB[:, :],
                                                   op0=ALU.mult, op1=ALU.mult),
           r=[xtok, tk, ("g", gkey)], w=[hntok])


def _transpose_to(sch, C, hn_ap_fn, hntok, pb, pbi, dstT, dsttok, col0, evac_eng="act"):
    ps = pb[pbi]
    ptok = ("pb", pbi)
    for cc in range(8):
        sch.op("pe", lambda e, cc=cc: e.transpose(out=ps[:, cc * 128:(cc + 1) * 128],
                                                  in_=hn_ap_fn(cc), identity=C.ident[:, :]),
               r=[hntok, ("ident",)], w=[ptok])
    src = ps[:, :].rearrange("p (c t) -> p c t", c=8)
    dst = dstT[:, :, col0:col0 + 128]
    if evac_eng == "act":
        sch.op("act", lambda e: e.copy(out=dst, in_=src), r=[ptok], w=[dsttok])
    else:
        sch.op("dve", lambda e: e.tensor_copy(out=dst, in_=src), r=[ptok], w=[dsttok])


def _load_weight_cols(sch, dst3, src2, c0, c1, tok, keyi, dst_c0=None):
    if dst_c0 is None:
        dst_c0 = c0
    srcv = src2.rearrange("(kc p) n -> p kc n", p=128)[:, :, c0:c1]
    dstv = dst3[:, :, dst_c0:dst_c0 + (c1 - c0)]
    sch.op("pool", lambda e: e.dma_start(out=dstv, in_=srcv), w=[tok], dma="w%d" % (keyi % 8))


def ffn_stage(nc, sch, C, name, xin, xout, wgu_d, wdn_d, grow, frow):
    with ExitStack() as es:
        T = lambda n, shp, dt: es.enter_context(nc.sbuf_tensor(name + n, shp, dt))
        wgu = T("wgu", [128, 8, 2 * DFF], BF16)
        wdn = T("wdn", [128, 22, D], BF16)
        gB = T("gB", [128, D], F32)
        gF = T("gF", [128, D], F32) if frow is not None else None
        xt = T("xt", [128, 4, D], F32)
        hn = T("hn", [128, 4, D], BF16)
        hT = T("hT", [128, 8, 512], BF16)
        aT = T("aT", [128, 22, 512], BF16)
        sg = T("sg", [128, 2, 512], F32)
        pf, pb = _psum_banks(nc, es, 6, 2)
        psA, psB, psC = pf[0:2], pf[2:4], pf[4:6]

        _load_gain(sch, C, gB, grow, "B")
        if frow is not None:
            _load_gain(sch, C, gF, frow, "F")
        def load_weights():
            ki = 0
            for j in range(4):
                _load_weight_cols(sch, wgu, wgu_d, j * 704, (j + 1) * 704, ("wgu", j), ki); ki += 1
                _load_weight_cols(sch, wgu, wgu_d, DFF + j * 704, DFF + (j + 1) * 704, ("wgu", j), ki); ki += 1
            for j in range(11):
                srcv = wdn_d.rearrange("(fc p) n -> p fc n", p=128)[:, 2 * j:2 * j + 2, :]
                dstv = wdn[:, 2 * j:2 * j + 2, :]
                sch.op("pool", lambda e, dstv=dstv, srcv=srcv: e.dma_start(out=dstv, in_=srcv),
                       w=[("wdn", j)], dma="w%d" % (ki % 8)); ki += 1

        cnt = {"x": 0, "k": 0}

        def load_x(src_d, t):
            slot = cnt["x"] % 4
            cnt["x"] += 1
            sch.op("sp", lambda e: e.dma_start(out=xt[:, slot, :], in_=src_d[t * 128:(t + 1) * 128, :]),
                   w=[("xt", slot)], dma="xl%d" % slot)
            return slot

        def phaseA_norm(ci):
            for s in range(4):
                t = ci * 4 + s
                slot = load_x(xin, t)
                k = ci * 4 + s
                hnap = hn[:, k % 4, :]
                _norm_to_hn(sch, C, xt[:, slot, :], ("xt", slot), gB, "B", hnap, ("hn", k % 4), k)

        def phaseA_tr(ci):
            for s in range(4):
                k = ci * 4 + s
                _transpose_to(sch, C, lambda cc, k=k: hn[:, k % 4, cc * 128:(cc + 1) * 128], ("hn", k % 4),
                              pb, k % 2, hT, ("hT",), s * 128)

        def phaseB(ci):
            for fi in range(22):
                pa, pbk = psA[fi % 2], psB[fi % 2]
                ta, tb_ = ("pf", fi % 2), ("pf", 2 + fi % 2)
                for cc in range(8):
                    sch.op("pe", lambda e, cc=cc, pa=pa, fi=fi: e.matmul(
                        pa[:, :], lhsT=wgu[:, cc, fi * 128:(fi + 1) * 128], rhs=hT[:, cc, :],
                        start=(cc == 0), stop=(cc == 7)),
                           r=[("wgu", (fi * 128) // 704), ("wgu", (fi * 128 + 127) // 704), ("hT",)], w=[ta])
                for cc in range(8):
                    sch.op("pe", lambda e, cc=cc, pbk=pbk, fi=fi: e.matmul(
                        pbk[:, :], lhsT=wgu[:, cc, DFF + fi * 128:DFF + (fi + 1) * 128], rhs=hT[:, cc, :],
                        start=(cc == 0), stop=(cc == 7)),
                           r=[("wgu", (fi * 128) // 704), ("wgu", (fi * 128 + 127) // 704), ("hT",)], w=[tb_])
                sgap = sg[:, fi % 2, :]
                sch.op("act", lambda e, pa=pa, sgap=sgap: e.activation(out=sgap, in_=pa[:, :], func=AF.Silu),
                       r=[ta], w=[("sg", fi % 2)])
                sch.op("dve", lambda e, pbk=pbk, sgap=sgap, fi=fi: e.tensor_tensor(
                    out=aT[:, fi, :], in0=sgap, in1=pbk[:, :], op=ALU.mult),
                       r=[("sg", fi % 2), tb_], w=[("aT", fi)])

        def phaseC(ci):
            for s in range(4):
                t = ci * 4 + s
                slot = load_x(xin, t)
                xr = xt[:, slot, :]
                for half in range(2):
                    pc = psC[half]
                    tc_ = ("pf", 4 + half)
                    for fi in range(22):
                        sch.op("pe", lambda e, fi=fi, pc=pc, half=half, s=s: e.matmul(
                            pc[:, :], lhsT=aT[:, fi, s * 128:(s + 1) * 128],
                            rhs=wdn[:, fi, half * 512:(half + 1) * 512],
                            start=(fi == 0), stop=(fi == 21)), r=[("aT", fi), ("wdn", fi // 2)], w=[tc_])
                    xh = xt[:, slot, half * 512:(half + 1) * 512]
                    sch.op("dve", lambda e, pc=pc, xh=xh: e.scalar_tensor_tensor(
                        out=xh, in0=pc[:, :], scalar=0.5, in1=xh, op0=ALU.mult, op1=ALU.add),
                           r=[tc_, ("xt", slot)], w=[("xt", slot)])
                if frow is not None:
                    k = cnt["k"]; cnt["k"] += 1
                    b = (k % 4) * 3
                    sa, sb, sc = C.st[:, b:b + 1], C.st[:, b + 1:b + 2], C.st[:, b + 2:b + 3]
                    tk = ("st", k % 4)
                    junk = sg[:, :, :].rearrange("p a b -> p (a b)")
                    sch.op("act", lambda e, xr=xr, sa=sa, junk=junk: e.activation(
                        out=junk, in_=xr, func=AF.Square, accum_out=sa),
                           r=[("xt", slot)], w=[("sg", 0), ("sg", 1), tk])
                    sch.op("dve", lambda e, sa=sa, sb=sb: e.tensor_scalar(
                        out=sb, in0=sa, scalar1=1.0 / D, scalar2=EPS, op0=ALU.mult, op1=ALU.add), r=[tk], w=[tk])
                    sch.op("pool", lambda e, sb=sb, sc=sc: e.tensor_tensor(
                        out=sc, in0=sb, in1=C.mhalf[:, 0:1], op=ALU.pow), r=[tk, ("mhalf",)], w=[tk])
                    sch.op("dve", lambda e, xr=xr, sc=sc: e.scalar_tensor_tensor(
                        out=xr, in0=xr, scalar=sc, in1=gF[:, :], op0=ALU.mult, op1=ALU.mult),
                           r=[("xt", slot), tk, ("g", "F")], w=[("xt", slot)])
                sch.op("sp", lambda e, xr=xr, t=t: e.dma_start(out=xout[t * 128:(t + 1) * 128, :], in_=xr),
                       r=[("xt", slot)], w=[("xout", t)], dma="xs%d" % slot)

        cnt["k"] = 64
        phaseA_norm(0)
        load_weights()
        phaseA_tr(0)
        if True:
            phaseA_norm(1)
        phaseB(0)
        for ci in range(8):
            if ci + 1 < 8:
                phaseA_tr(ci + 1)
            phaseC(ci)
            if ci + 2 < 8:
                phaseA_norm(ci + 2)
            if ci + 1 < 8:
                phaseB(ci + 1)
        sch.emit_stage()


def inproj_stage(nc, sch, C):
    with ExitStack() as es:
        T = lambda n, shp, dt: es.enter_context(nc.sbuf_tensor("ip" + n, shp, dt))
        win = T("win", [128, 8, 3072], BF16)
        gB = T("gB", [128, D], F32)
        xt = T("xt", [128, 4, D], F32)
        hn = T("hn", [128, 4, D], BF16)
        hT = T("hT", [128, 8, 512], BF16)
        qk = T("qk", [128, 2, 22, 512], BF16)
        vt = T("vt", [128, 2, 4, 4, 65], BF16)
        pf, pb = _psum_banks(nc, es, 6, 2)

        _load_gain(sch, C, gB, 1, "B")
        sch.op("pool", lambda e: e.memset(vt[:, :, :, :, 64:65], 1.0), w=[("vt", 0), ("vt", 1)])

        fcs = [0, 1, 2, 3, 4, 5, 18, 19, 6, 7, 8, 9, 10, 11, 20, 21, 12, 13, 14, 15, 16, 17]
        qTv = C.qT.rearrange("(i p) t -> p i t", p=128)
        kTv = C.kT.rearrange("(i p) t -> p i t", p=128)
        vTv = C.vT.rearrange("(i p) t -> p i t", p=128)
        Vv = C.V.rearrange("h p t e -> p h t e")
        cnt = {"x": 0, "k": 0, "pf": 0}

        def phaseA_norm(ci):
            for s in range(4):
                t = ci * 4 + s
                slot = cnt["x"] % 4; cnt["x"] += 1
                sch.op("sp", lambda e, slot=slot, t=t: e.dma_start(out=xt[:, slot, :],
                                                                  in_=C.x1[t * 128:(t + 1) * 128, :]),
                       w=[("xt", slot)], dma="xl%d" % slot)
                k = ci * 4 + s
                _norm_to_hn(sch, C, xt[:, slot, :], ("xt", slot), gB, "B", hn[:, k % 4, :], ("hn", k % 4), k)

        def phaseA_tr(ci):
            for s in range(4):
                k = ci * 4 + s
                _transpose_to(sch, C, lambda cc, k=k: hn[:, k % 4, cc * 128:(cc + 1) * 128], ("hn", k % 4),
                              pb, k % 2, hT, ("hT",), s * 128)

        def phaseB(ci):
            sl = ci % 2
            for i, fc in enumerate(fcs):
                bi = cnt["pf"] % 6; cnt["pf"] += 1
                ps = pf[bi]
                for cc in range(8):
                    sch.op("pe", lambda e, cc=cc, ps=ps, fc=fc: e.matmul(
                        ps[:, :], lhsT=win[:, cc, fc * 128:(fc + 1) * 128], rhs=hT[:, cc, :],
                        start=(cc == 0), stop=(cc == 7)), r=[("win", fc // 2), ("hT",)], w=[("pf", bi)])
                dst = qk[:, sl, i, :]
                if i % 2 == 0:
                    sch.op("act", lambda e, ps=ps, dst=dst: e.copy(out=dst, in_=ps[:, :]),
                           r=[("pf", bi)], w=[("qk", sl, i)])
                else:
                    sch.op("dve", lambda e, ps=ps, dst=dst: e.tensor_copy(out=dst, in_=ps[:, :]),
                           r=[("pf", bi)], w=[("qk", sl, i)])
            for (dv, base, lo, n, nm) in ((qTv, 0, 0, 4, "q0"), (qTv, 0, 4, 4, "q1"), (kTv, 8, 0, 4, "k0"),
                                          (kTv, 8, 4, 4, "k1"), (vTv, 16, 0, 3, "v0"), (vTv, 16, 3, 3, "v1")):
                sch.op("sp", lambda e, sl=sl, ci=ci, dv=dv, base=base, lo=lo, n=n: e.dma_start(
                    out=dv[:, lo:lo + n, ci * 512:(ci + 1) * 512], in_=qk[:, sl, base + lo:base + lo + n, :]),
                       r=[("qk", sl, i) for i in range(base + lo, base + lo + n)], w=[("st" + nm, ci)],
                       dma="qs%s%d" % (nm, sl))
            for s in range(4):
                bi = cnt["pf"] % 6; cnt["pf"] += 1
                ps = pf[bi]
                for cc in range(8):
                    sch.op("pe", lambda e, cc=cc, ps=ps, s=s: e.matmul(
                        ps[:, 0:256], lhsT=hT[:, cc, s * 128:(s + 1) * 128], rhs=win[:, cc, 2816:3072],
                        start=(cc == 0), stop=(cc == 7)),
                           r=[("win", 11), ("hT",)], w=[("pf", bi)])
                src = ps[:, 0:256].rearrange("p (h e) -> p h e", e=64)
                dst = vt[:, sl, :, s, 0:64]
                sch.op("dve", lambda e, src=src, dst=dst: e.tensor_copy(out=dst, in_=src),
                       r=[("pf", bi)], w=[("vtw", sl, s)])
            sch.op("sp", lambda e, sl=sl, ci=ci: e.dma_start(
                out=Vv[:, :, ci * 4:(ci + 1) * 4, :], in_=vt[:, sl, :, :, :]),
                   r=[("vt", sl)] + [("vtw", sl, s) for s in range(4)],
                   w=[("V", ci)], dma="vs%d" % sl)

        phaseA_norm(0)
        for j in range(12):
            _load_weight_cols(sch, win, C.w_in, j * 256, (j + 1) * 256, ("win", j), j)
        phaseA_tr(0)
        for ci in range(8):
            if ci + 1 < 8:
                phaseA_norm(ci + 1)
            phaseB(ci)
            if ci + 1 < 8:
                phaseA_tr(ci + 1)
        sch.emit_stage()


def attn_stage(nc, sch, C, heads=range(16)):
    with ExitStack() as es:
        T = lambda n, shp, dt: es.enter_context(nc.sbuf_tensor("at" + n, shp, dt))
        qt = T("qt", [128, 2, S], BF16)
        kt = T("kt", [128, 2, S], BF16)
        vtT = T("vtT", [128, 2, S], BF16)
        vt = T("vt", [128, 2, 32, 65], BF16)
        tbs = T("tbs", [128, 2, 3, 256], F32)
        cms = T("cms", [128, 256], F32)
        Ws = T("Ws", [128, 2, 3, 256], BF16)
        vk = T("vk", [128, 3, 65], BF16)
        acc = T("acc", [128, S], F32)
        tbm = T("tbm", [128, 2, NW], F32)
        cm = T("cm", [128, NW], F32)
        Wm = T("Wm", [128, 2, NW], BF16)
        Eb = T("Eb", [128, 3, 512], BF16)
        Pb = T("Pb", [128, 3, 512], BF16)
        oh = T("oh", [128, 2, 32, 64], F32)
        km = T("km", [128, 80], BF16)
        kmf = T("kmf", [128, 16], F32)
        gmf = T("gmf", [128, 4, 16], F32)
        m8 = T("m8", [128, 4, 8], F32)
        selin = T("selin", [128, 4, 80], F32)
        mcon = T("mcon", [128, 16, 16], F32)
        rd = T("rd", [128, 8], F32)
        idf = T("idf", [128, 128], F32)
        psS = [es.enter_context(nc.psum_tensor("psS%d" % i, [128, 512], F32)) for i in range(2)]
        psO = [es.enter_context(nc.psum_tensor("psO%d" % i, [128, 512], F32)) for i in range(4)]
        psM = es.enter_context(nc.psum_tensor("psM", [128, 512], F32))
        psVb = es.enter_context(nc.psum_tensor("psVb", [128, 1024], BF16))
        psG = psM

        sch.op("sp", lambda e: e.dma_start(out=idf[:, :], in_=C.identf), w=[("idf",)], dma="c2")
        sch.op("sp", lambda e: e.dma_start(out=cm[:, :], in_=C.cmask), w=[("cm",)], dma="c0")
        sch.op("sp", lambda e: e.dma_start(out=cms[:, :], in_=C.cms), w=[("cms",)], dma="c3")
        sch.op("sp", lambda e: e.dma_start(out=mcon[:, :, :], in_=C.mconst), w=[("mcon",)], dma="c1")
        for sl in range(2):
            sch.op("pool", lambda e, sl=sl: e.memset(kt[64:128, sl, :], 0.0), w=[("ktaug", sl)])
            sch.op("pool", lambda e, sl=sl: e.memset(qt[64:128, sl, :], 0.0), w=[("qtz", sl)])
            sch.op("pool", lambda e, sl=sl: e.memset(vtT[64:128, sl, :], 0.0), w=[("vtz", sl)])
            sch.op("pool", lambda e, sl=sl: e.dma_start(
                out=kt[64:80, sl, :].rearrange("j (a b) -> j a b", b=2048),
                in_=C.kbind.rearrange("j (a b) -> j a b", b=2048)),
                   w=[("ktaug", sl)], dma="w%d" % sl)
        sch.op("pool", lambda e: e.memset(km[:, :], 0.0), w=[("km",)])
        sch.op("pool", lambda e: e.memset(selin[:, :, :], 0.0), w=[("selin", 0), ("selin", 1), ("selin", 2), ("selin", 3)])
        sch.op("pool", lambda e: e.memset(vk[:, :, 64:65], 1.0), w=[("vk", 0), ("vk", 1), ("vk", 2)])
        Ov = C.O.rearrange("t p f -> p t f")

        def load_head(h):
            sl = h % 2
            sch.op("sp", lambda e: e.dma_start(out=qt[0:64, sl, :], in_=C.qT[h * 64:(h + 1) * 64, :]),
                   w=[("qt", sl)], dma="hq%d" % sl)
            sch.op("sp", lambda e: e.dma_start(out=kt[0:64, sl, :], in_=C.kT[h * 64:(h + 1) * 64, :]),
                   w=[("kt", sl)], dma="hk%d" % sl)
            if h < 12:
                sch.op("sp", lambda e: e.dma_start(out=vtT[0:64, sl, :], in_=C.vT[h * 64:(h + 1) * 64, :]),
                       w=[("vtT", sl)], dma="hv%d" % sl)
                sch.op("sp", lambda e: e.dma_start(out=tbs[:, sl, :, :], in_=C.tbs[h]),
                       w=[("tbs", sl)], dma="ht%d" % sl)
            else:
                sch.op("sp", lambda e: e.dma_start(out=vt[:, sl, :, :], in_=C.V[h - 12]),
                       w=[("vth", sl)], dma="hv%d" % sl)
                sch.op("sp", lambda e: e.dma_start(out=tbm[:, sl, :], in_=C.tb[h - 12]),
                       w=[("tbm", sl)], dma="ht%d" % sl)

        def dilated_head(h, sl):
            sch.op("act", lambda e: e.activation(out=tbs[:, sl, :, :], in_=tbs[:, sl, :, :], func=AF.Exp),
                   r=[("tbs", sl)], w=[("tbs", sl)])
            for di in range(3):
                sch.op("dve", lambda e, di=di: e.tensor_tensor(out=Ws[:, sl, di, :], in0=tbs[:, sl, di, :],
                                                             in1=cms[:, :], op=ALU.mult),
                       r=[("tbs", sl), ("cms",)], w=[("Ws", sl)])
            items = []
            for di, d in enumerate((1, 4, 16)):
                nt_ = (S // d) // 128
                for r in range(d):
                    for jt in range(nt_):
                        items.append((di, d, r, jt, nt_))
            n = len(items)

            psS3 = [psS[0], psS[1], psO[3]]
            psS3tok = [("psS", 0), ("psS", 1), ("psO", 3)]

            def front(i):
                di, d, r, jt, nt_ = items[i]
                b = i % 3
                b3 = i % 3
                nq = 256 if jt + 1 < nt_ else 128
                ks = jt * 128 * d + r
                lhs = kt[0:128, sl, ks:ks + 127 * d + 1:d]
                rhs = qt[0:128, sl, ks:ks + (nq - 1) * d + 1:d]
                pS = psS3[b]
                sch.op("pe", lambda e, lhs=lhs, rhs=rhs, pS=pS, nq=nq: e.matmul(
                    pS[:, 0:nq], lhsT=lhs, rhs=rhs, start=True, stop=True),
                       r=[("kt", sl), ("ktaug", sl), ("qt", sl), ("qtz", sl)], w=[psS3tok[b]])
                eb = Eb[:, b3, 0:nq]
                sch.op("act", lambda e, eb=eb, pS=pS, nq=nq: e.activation(out=eb, in_=pS[:, 0:nq], func=AF.Exp,
                                                                       scale=0.125),
                       r=[psS3tok[b]], w=[("Eb", b3)])
                pbv = Pb[:, b3, 0:nq]
                wv = Ws[:, sl, di, 0:nq]
                sch.op("pool" if i % 2 == 1 else "dve",
                       lambda e, pbv=pbv, eb=eb, wv=wv: e.tensor_tensor(out=pbv, in0=eb, in1=wv, op=ALU.mult),
                       r=[("Eb", b3), ("Ws", sl)], w=[("Pb", b3)])
                vin = vtT[0:128, sl, ks:ks + 127 * d + 1:d]
                sch.op("pe", lambda e, vin=vin: e.transpose(out=psVb[:, 0:128], in_=vin, identity=C.ident[:, :]),
                       r=[("vtT", sl), ("vtz", sl), ("ident",)], w=[("psVb",)])
                vkv = vk[:, b3, 0:64]
                sch.op("act", lambda e, vkv=vkv: e.copy(out=vkv, in_=psVb[:, 0:64]),
                       r=[("psVb",)], w=[("vk", b3)])

            def back(i):
                di, d, r, jt, nt_ = items[i]
                b3 = i % 3
                for qi in range(2):
                    jq = jt + qi
                    if jq >= nt_:
                        continue
                    bank = jq % 2
                    po = psO[bank][0:65, 0:128]
                    lhs = vk[:, b3, 0:65]
                    rhs = Pb[:, b3, qi * 128:(qi + 1) * 128]
                    st_ = (qi == 1) or (jq == 0)
                    sp_ = (qi == 0)
                    sch.op("pe", lambda e, po=po, lhs=lhs, rhs=rhs, st_=st_, sp_=sp_: e.matmul(
                        po, lhsT=lhs, rhs=rhs, start=st_, stop=sp_),
                           r=[("vk", b3), ("Pb", b3)], w=[("psO", bank)])
                    if sp_:
                        qs = jq * 128 * d + r
                        av = acc[0:65, qs:qs + 127 * d + 1:d]
                        if di == 0:
                            sch.op("act", lambda e, av=av, po=po: e.copy(out=av, in_=po),
                                   r=[("psO", bank)], w=[("acc",)])
                        else:
                            sch.op("dve", lambda e, av=av, po=po: e.tensor_tensor(out=av, in0=av, in1=po, op=ALU.add),
                                   r=[("psO", bank), ("acc",)], w=[("acc",)])

            for i in range(n + 2):
                if i < n:
                    front(i)
                if i >= 2:
                    back(i - 2)
            for t in range(NT):
                s4 = t % 4
                pF = psO[2 + t % 2]
                sch.op("pe", lambda e, t=t, pF=pF: e.transpose(out=pF[:, 0:65], in_=acc[0:65, t * 128:(t + 1) * 128],
                                                           identity=idf[0:65, 0:65]),
                       r=[("acc",), ("idf",)], w=[("psO", 2 + t % 2)])
                sch.op("dve", lambda e, s4=s4, pF=pF: e.reciprocal(out=rd[:, s4:s4 + 1], in_=pF[:, 64:65]),
                       r=[("psO", 2 + t % 2)], w=[("rd", s4)])
                sch.op("dve", lambda e, s4=s4, pF=pF, t=t: e.tensor_scalar(
                    out=oh[:, sl, t, :], in0=pF[:, 0:64], scalar1=rd[:, s4:s4 + 1], scalar2=None, op0=ALU.mult),
                       r=[("psO", 2 + t % 2), ("rd", s4)], w=[("oh", sl, t)])
                if s4 == 3:
                    qc = t // 4
                    sch.op("sp", lambda e, qc=qc: e.dma_start(
                        out=Ov[:, qc * 4:(qc + 1) * 4, h * 64:(h + 1) * 64], in_=oh[:, sl, qc * 4:(qc + 1) * 4, :]),
                           r=[("oh", sl, qc * 4 + s) for s in range(4)], w=[("O", h, qc)], dma="os%d" % (qc % 4))

        def moba_head(h, sl):
            K = 128
            sch.op("act", lambda e: e.activation(out=tbm[:, sl, :], in_=tbm[:, sl, :], func=AF.Exp),
                   r=[("tbm", sl)], w=[("tbm", sl)])
            sch.op("dve", lambda e: e.tensor_tensor(out=Wm[:, sl, :], in0=tbm[:, sl, :], in1=cm[:, :], op=ALU.mult),
                   r=[("tbm", sl), ("cm",)], w=[("Wm", sl)])
            ktok = [("kt", sl), ("ktaug", sl)]
            sch.op("dve", lambda e: e.tensor_reduce(
                out=kmf[0:64, :], in_=kt[0:64, sl, :].rearrange("p (a b) -> p a b", b=256), axis=AX.X, op=ALU.add),
                   r=[("kt", sl)], w=[("kmf",)])
            sch.op("dve", lambda e: e.tensor_copy(out=km[0:64, 64:80], in_=kmf[0:64, :]),
                   r=[("kmf",)], w=[("km",)])
            gbanks = [psS[0], psS[1], psO[0], psO[1], psO[2], psO[3]]
            gtoks = [("psS", 0), ("psS", 1), ("psO", 0), ("psO", 1), ("psO", 2), ("psO", 3)]

            def gate_front(t):
                n = t // 2
                g = t % 4
                bk, bt = gbanks[t % 6], gtoks[t % 6]
                sch.op("pe", lambda e: e.matmul(
                    bk[:, 0:80], lhsT=qt[0:128, sl, t * 128:(t + 1) * 128], rhs=km[0:128, 0:80],
                    start=True, stop=True), r=[("qt", sl), ("qtz", sl), ("km",)], w=[bt])
                sch.op("dve", lambda e: e.tensor_tensor(
                    out=gmf[:, g, :], in0=bk[:, 64:80], in1=mcon[:, n, :], op=ALU.add),
                       r=[bt, ("mcon",)], w=[("gmf", g)])
                sch.op("dve", lambda e: e.max(out=m8[:, g, :], in_=gmf[:, g, :]),
                       r=[("gmf", g)], w=[("m8", g)])
                sch.op("dve", lambda e: e.tensor_scalar(
                    out=selin[:, g, 64:80], in0=gmf[:, g, :], scalar1=m8[:, g, 3:4], scalar2=1.0,
                    op0=ALU.is_ge, op1=ALU.subtract), r=[("gmf", g), ("m8", g)], w=[("selin", g)])

            def gate_back(t):
                g = t % 4
                bk, bt = gbanks[t % 6], gtoks[t % 6]
                sch.op("pe", lambda e: e.transpose(out=bk[0:80, 128:256], in_=selin[:, g, 0:80],
                                                   identity=idf[:, :]),
                       r=[("selin", g), ("idf",)], w=[bt])
                sch.op("act", lambda e: e.copy(out=qt[64:80, sl, t * 128:(t + 1) * 128],
                                               in_=bk[64:80, 128:256]),
                       r=[bt], w=[("qtaug", sl, t)])

            for t in range(NT + 2):
                if t < NT:
                    gate_front(t)
                if t >= 2:
                    gate_back(t - 2)
            for qc in range(8):
                ktl = list(range(0, qc * 4 + 4))
                nk = len(ktl)

                def valid(kti, s, qc=qc):
                    return kti * 128 <= qc * 512 + s * 128 + 127

                firsts = {s: min(k for k in ktl if valid(k, s)) for s in range(4)}
                lasts = {s: max(k for k in ktl if valid(k, s)) for s in range(4)}
                qr = [("qt", sl), ("qtz", sl)] + [("qtaug", sl, qc * 4 + s) for s in range(4)]

                mS3 = [psS[0], psS[1], psM]
                mS3tok = [("psS", 0), ("psS", 1), ("psM",)]

                def QK(i, qc=qc, ktl=ktl, qr=qr):
                    kti = ktl[i]
                    b3 = i % 3
                    lhs = kt[0:K, sl, kti * 128:(kti + 1) * 128]
                    rhs = qt[0:K, sl, qc * 512:(qc + 1) * 512]
                    pS = mS3[b3]
                    sch.op("pe", lambda e, lhs=lhs, rhs=rhs, pS=pS: e.matmul(
                        pS[:, :], lhsT=lhs, rhs=rhs, start=True, stop=True),
                           r=ktok + qr, w=[mS3tok[b3]])
                    eb = Eb[:, b3, :]
                    sch.op("act", lambda e, eb=eb, pS=pS: e.activation(out=eb, in_=pS[:, :], func=AF.Exp, scale=0.125),
                           r=[mS3tok[b3]], w=[("Eb", b3)])
                    off = min(qc * 512 - kti * 128, 2048) + WOFF
                    pbv = Pb[:, b3, :]
                    wv = Wm[:, sl, off:off + 512]
                    sch.op("pool" if i % 3 == 1 else "dve",
                           lambda e, pbv=pbv, eb=eb, wv=wv: e.tensor_tensor(out=pbv, in0=eb, in1=wv, op=ALU.mult),
                           r=[("Eb", b3), ("Wm", sl)], w=[("Pb", b3)])

                def PV(i, ktl=ktl, firsts=firsts, lasts=lasts, valid=valid):
                    kti = ktl[i]
                    b3 = i % 3
                    for s in range(4):
                        if not valid(kti, s):
                            continue
                        po = psO[s][:, 0:65]
                        lhs = Pb[:, b3, s * 128:(s + 1) * 128]
                        rhs = vt[:, sl, kti, :]
                        st_ = (kti == firsts[s])
                        sp_ = (kti == lasts[s])
                        sch.op("pe", lambda e, po=po, lhs=lhs, rhs=rhs, st_=st_, sp_=sp_: e.matmul(
                            po, lhsT=lhs, rhs=rhs, start=st_, stop=sp_),
                               r=[("Pb", b3), ("vth", sl)], w=[("psO", s)])

                for i in range(nk + 2):
                    if i < nk:
                        QK(i)
                    if i >= 2:
                        PV(i - 2)
                for s in range(4):
                    t = qc * 4 + s
                    sch.op("dve", lambda e, s=s: e.reciprocal(out=rd[:, 4 + s:5 + s], in_=psO[s][:, 64:65]),
                           r=[("psO", s)], w=[("rd", 4 + s)])
                    sch.op("dve", lambda e, s=s, t=t: e.tensor_scalar(
                        out=oh[:, sl, t, :], in0=psO[s][:, 0:64], scalar1=rd[:, 4 + s:5 + s], scalar2=None,
                        op0=ALU.mult), r=[("psO", s), ("rd", 4 + s)], w=[("oh", sl, t)])
                sch.op("sp", lambda e, qc=qc: e.dma_start(
                    out=Ov[:, qc * 4:(qc + 1) * 4, h * 64:(h + 1) * 64], in_=oh[:, sl, qc * 4:(qc + 1) * 4, :]),
                       r=[("oh", sl, qc * 4 + s) for s in range(4)], w=[("O", h, qc)], dma="os%d" % (qc % 4))

        hl = list(heads)
        load_head(hl[0])
        for hi, h in enumerate(hl):
            sl = h % 2
            if hi + 1 < len(hl):
                load_head(hl[hi + 1])
            if h < 12:
                dilated_head(h, sl)
            else:
                moba_head(h, sl)
        sch.emit_stage()


def cross_stage(nc, sch, C):
    with ExitStack() as es0:
        T0 = lambda n, shp, dt: es0.enter_context(nc.sbuf_tensor("cr" + n, shp, dt))
        kcT = T0("kcT", [128, 8, 256], BF16)
        vc = T0("vc", [128, 2, D], BF16)
        with ExitStack() as es:
            T = lambda n, shp, dt: es.enter_context(nc.sbuf_tensor("cm" + n, shp, dt))
            wckv = T("wckv", [128, 8, 2 * D], BF16)
            gM = T("gM", [128, D], F32)
            mt_ = T("mt", [128, 2, D], F32)
            hn = T("hn", [128, 2, D], BF16)
            hT = T("hT", [128, 8, 256], BF16)
            pf, pb = _psum_banks(nc, es, 6, 2)
            _load_gain(sch, C, gM, 4, "M")
            for mt in range(2):
                sch.op("sp", lambda e, mt=mt: e.dma_start(out=mt_[:, mt, :], in_=C.mem[mt * 128:(mt + 1) * 128, :]),
                       w=[("mt", mt)], dma="ol%d" % mt)
                _norm_to_hn(sch, C, mt_[:, mt, :], ("mt", mt), gM, "M", hn[:, mt, :], ("hn", mt), mt)
            for j in range(8):
                _load_weight_cols(sch, wckv, C.w_ckv, j * 256, (j + 1) * 256, ("wckv", j), j)
            for mt in range(2):
                _transpose_to(sch, C, lambda cc, mt=mt: hn[:, mt, cc * 128:(cc + 1) * 128], ("hn", mt),
                              pb, mt, hT, ("hT",), mt * 128)
            nb = [0]

            def nextpf0():
                bi = nb[0] % 6
                nb[0] += 1
                return bi
            for fc in range(8):
                bi = nextpf0()
                for cc in range(8):
                    sch.op("pe", lambda e, cc=cc, fc=fc, bi=bi: e.matmul(
                        pf[bi][:, 0:256], lhsT=wckv[:, cc, fc * 128:(fc + 1) * 128], rhs=hT[:, cc, 0:256],
                        start=(cc == 0), stop=(cc == 7)), r=[("wckv", fc // 2), ("hT",)], w=[("pf", bi)])
                sch.op("dve", lambda e, fc=fc, bi=bi: e.tensor_copy(out=kcT[:, fc, :], in_=pf[bi][:, 0:256]),
                       r=[("pf", bi)], w=[("kcT",)])
            for mt in range(2):
                for half in range(2):
                    bi = nextpf0()
                    for cc in range(8):
                        sch.op("pe", lambda e, cc=cc, mt=mt, half=half, bi=bi: e.matmul(
                            pf[bi][:, :], lhsT=hT[:, cc, mt * 128:(mt + 1) * 128],
                            rhs=wckv[:, cc, D + half * 512:D + (half + 1) * 512],
                            start=(cc == 0), stop=(cc == 7)),
                               r=[("wckv", 4 + half * 2), ("wckv", 5 + half * 2), ("hT",)], w=[("pf", bi)])
                    sch.op("dve", lambda e, mt=mt, half=half, bi=bi: e.tensor_copy(
                        out=vc[:, mt, half * 512:(half + 1) * 512], in_=pf[bi][:, :]),
                           r=[("pf", bi)], w=[("vc",)])
            sch.emit_stage()

        with ExitStack() as es:
            T = lambda n, shp, dt: es.enter_context(nc.sbuf_tensor("cr" + n, shp, dt))
            wout = T("wout", [128, 8, D], BF16)
            wcq = T("wcq", [128, 8, D], BF16)
            wco = T("wco", [128, 8, D], BF16)
            gO = T("gO", [128, D], F32)
            gC = T("gC", [128, D], F32)
            ot = T("ot", [128, 2, D], F32)
            x1t = T("x1t", [128, 4, D], F32)
            x2 = T("x2", [128, 2, 4, D], F32)
            hn = T("hn", [128, 4, D], BF16)
            yT = T("yT", [128, 8, 512], BF16)
            hT = T("hT", [128, 2, 8, 512], BF16)
            qcT = T("qcT", [128, 8, 512], BF16)
            Pc = T("Pc", [128, 2, 2, 512], BF16)
            rdt = T("rdt", [128, 2, 512], F32)
            ocT = T("ocT", [128, 8, 512], BF16)
            pf, pb = _psum_banks(nc, es, 6, 2)
            cnt = {"pf": 0, "k": 0}

            def nextpf():
                bi = cnt["pf"] % 6
                cnt["pf"] += 1
                return bi

            _load_gain(sch, C, gO, 2, "O")
            _load_gain(sch, C, gC, 3, "C")

            def load_w():
                ki = 0
                for (wt, wd, nm) in ((wout, C.w_out, "wout"), (wcq, C.w_cq, "wcq"), (wco, C.w_co, "wco")):
                    for j in range(4):
                        _load_weight_cols(sch, wt, wd, j * 256, (j + 1) * 256, (nm, j), ki); ki += 1

            def P1_norm(ci):
                ks = []
                for s in range(4):
                    t = ci * 4 + s
                    sl = t % 2
                    sch.op("sp", lambda e, sl=sl, t=t: e.dma_start(out=ot[:, sl, :], in_=C.O[t]),
                           w=[("ot", sl)], dma="ol%d" % sl)
                    sch.op("sp", lambda e, s=s, t=t: e.dma_start(out=x1t[:, s, :],
                                                                in_=C.x1[t * 128:(t + 1) * 128, :]),
                           w=[("x1t", s)], dma="xl%d" % s)
                    k = cnt["k"]; cnt["k"] += 1
                    ks.append(k)
                    b = (k % 4) * 3
                    st = C.st
                    sa, sb, sc = st[:, b:b + 1], st[:, b + 1:b + 2], st[:, b + 2:b + 3]
                    b2 = 12 + (k % 4) * 3
                    sa2, sb2, sc2 = st[:, b2:b2 + 1], st[:, b2 + 1:b2 + 2], st[:, b2 + 2:b2 + 3]
                    tk = ("st", k % 4)
                    kk = k % 4
                    sch.op("act", lambda e, sl=sl, kk=kk, sa=sa: e.activation(
                        out=hn[:, kk, 0:768], in_=ot[:, sl, 0:768], func=AF.Square, accum_out=sa),
                           r=[("ot", sl)], w=[("hn", kk), tk])
                    sch.op("act", lambda e, sl=sl, kk=kk, sa2=sa2: e.activation(
                        out=hn[:, kk, 768:1024], in_=ot[:, sl, 768:1024], func=AF.Square, accum_out=sa2),
                           r=[("ot", sl)], w=[("hn", kk), tk])
                    sch.op("dve", lambda e, sa=sa, sb=sb: e.tensor_scalar(
                        out=sb, in0=sa, scalar1=1.0 / 768, scalar2=EPS, op0=ALU.mult, op1=ALU.add), r=[tk], w=[tk])
                    sch.op("dve", lambda e, sa2=sa2, sb2=sb2: e.tensor_scalar(
                        out=sb2, in0=sa2, scalar1=1.0 / 256, scalar2=EPS, op0=ALU.mult, op1=ALU.add), r=[tk], w=[tk])
                    sch.op("pool", lambda e, sb=sb, sc=sc: e.tensor_tensor(
                        out=sc, in0=sb, in1=C.mhalf[:, 0:1], op=ALU.pow), r=[tk, ("mhalf",)], w=[tk])
                    sch.op("pool", lambda e, sb2=sb2, sc2=sc2: e.tensor_tensor(
                        out=sc2, in0=sb2, in1=C.mhalf[:, 0:1], op=ALU.pow), r=[tk, ("mhalf",)], w=[tk])
                    sch.op("dve", lambda e, sl=sl, kk=kk, sc=sc: e.scalar_tensor_tensor(
                        out=hn[:, kk, 0:768], in0=ot[:, sl, 0:768], scalar=sc, in1=gO[:, 0:768],
                        op0=ALU.mult, op1=ALU.mult), r=[("ot", sl), tk, ("g", "O")], w=[("hn", kk)])
                    sch.op("dve", lambda e, sl=sl, kk=kk, sc2=sc2: e.scalar_tensor_tensor(
                        out=hn[:, kk, 768:1024], in0=ot[:, sl, 768:1024], scalar=sc2, in1=gO[:, 768:1024],
                        op0=ALU.mult, op1=ALU.mult), r=[("ot", sl), tk, ("g", "O")], w=[("hn", kk)])
                    _transpose_to(sch, C, lambda cc, kk=kk: hn[:, kk, cc * 128:(cc + 1) * 128], ("hn", kk),
                                  pb, k % 2, yT, ("yT", s), s * 128)

            def P1_wout(ci):
                xs = ci % 2
                for s in range(4):
                    for half in range(2):
                        bi = nextpf()
                        for cc in range(8):
                            sch.op("pe", lambda e, cc=cc, s=s, half=half, bi=bi: e.matmul(
                                pf[bi][:, :], lhsT=yT[:, cc, s * 128:(s + 1) * 128],
                                rhs=wout[:, cc, half * 512:(half + 1) * 512], start=(cc == 0), stop=(cc == 7)),
                                   r=[("yT", s), ("wout", half * 2), ("wout", half * 2 + 1)], w=[("pf", bi)])
                        sch.op("dve", lambda e, s=s, xs=xs, half=half, bi=bi: e.tensor_tensor(
                            out=x2[:, xs, s, half * 512:(half + 1) * 512], in0=pf[bi][:, :],
                            in1=x1t[:, s, half * 512:(half + 1) * 512], op=ALU.add),
                               r=[("pf", bi), ("x1t", s)], w=[("x2", xs, s)])

            n2k = {}

            def P1_norm2(ci):
                xs = ci % 2
                for s in range(4):
                    k = cnt["k"]; cnt["k"] += 1
                    n2k[(ci, s)] = k
                    kk = k % 4
                    _norm_to_hn(sch, C, x2[:, xs, s, :], ("x2", xs, s), gC, "C", hn[:, kk, :], ("hn", kk), k)

            def P1_tr2(ci):
                xs = ci % 2
                for s in range(4):
                    k = n2k[(ci, s)]
                    kk = k % 4
                    _transpose_to(sch, C, lambda cc, kk=kk: hn[:, kk, cc * 128:(cc + 1) * 128], ("hn", kk),
                                  pb, k % 2, hT[:, xs], ("hT", xs), s * 128)

            def P2(ci):
                xs = ci % 2
                for fc in range(8):
                    bi = nextpf()
                    for cc in range(8):
                        sch.op("pe", lambda e, cc=cc, fc=fc, bi=bi, xs=xs: e.matmul(
                            pf[bi][:, :], lhsT=wcq[:, cc, fc * 128:(fc + 1) * 128], rhs=hT[:, xs, cc, :],
                            start=(cc == 0), stop=(cc == 7)), r=[("wcq", fc // 2), ("hT", xs)], w=[("pf", bi)])
                    sch.op("act", lambda e, fc=fc, bi=bi: e.copy(out=qcT[:, fc, :], in_=pf[bi][:, :]),
                           r=[("pf", bi)], w=[("qcT", fc)])

            def P3(ci):
                for hd in range(4):
                    pslot = hd % 2
                    for mt in range(2):
                        bi = nextpf()
                        for e2 in range(2):
                            sch.op("pe", lambda e, e2=e2, mt=mt, hd=hd, bi=bi: e.matmul(
                                pf[bi][:, :], lhsT=kcT[:, 2 * hd + e2, mt * 128:(mt + 1) * 128],
                                rhs=qcT[:, 2 * hd + e2, :], start=(e2 == 0), stop=(e2 == 1)),
                                   r=[("kcT",), ("qcT", 2 * hd + e2)], w=[("pf", bi)])
                        sch.op("act", lambda e, mt=mt, pslot=pslot, bi=bi: e.activation(
                            out=Pc[:, pslot, mt, :], in_=pf[bi][:, :], func=AF.Exp, scale=1.0 / 16.0),
                               r=[("pf", bi)], w=[("Pc", pslot, mt)])
                    bd = nextpf()
                    for mt in range(2):
                        sch.op("pe", lambda e, mt=mt, pslot=pslot, bd=bd: e.matmul(
                            pf[bd][:, :], lhsT=C.ones[:, :], rhs=Pc[:, pslot, mt, :],
                            start=(mt == 0), stop=(mt == 1)),
                               r=[("ones",), ("Pc", pslot, mt)], w=[("pf", bd)])
                    sch.op("dve", lambda e, pslot=pslot, bd=bd: e.reciprocal(out=rdt[:, pslot, :], in_=pf[bd][:, :]),
                           r=[("pf", bd)], w=[("rdt", pslot)])
                    for e2 in range(2):
                        bi = nextpf()
                        for mt in range(2):
                            sch.op("pe", lambda e, e2=e2, mt=mt, hd=hd, pslot=pslot, bi=bi: e.matmul(
                                pf[bi][:, :], lhsT=vc[:, mt, (2 * hd + e2) * 128:(2 * hd + e2 + 1) * 128],
                                rhs=Pc[:, pslot, mt, :], start=(mt == 0), stop=(mt == 1)),
                                   r=[("vc",), ("Pc", pslot, mt)], w=[("pf", bi)])
                        sch.op("dve", lambda e, e2=e2, hd=hd, pslot=pslot, bi=bi: e.tensor_tensor(
                            out=ocT[:, 2 * hd + e2, :], in0=pf[bi][:, :], in1=rdt[:, pslot, :], op=ALU.mult),
                               r=[("pf", bi), ("rdt", pslot)], w=[("ocT", 2 * hd + e2)])

            def P4(ci):
                xs = ci % 2
                for s in range(4):
                    t = ci * 4 + s
                    for half in range(2):
                        bi = nextpf()
                        for cc in range(8):
                            sch.op("pe", lambda e, cc=cc, s=s, half=half, bi=bi: e.matmul(
                                pf[bi][:, :], lhsT=ocT[:, cc, s * 128:(s + 1) * 128],
                                rhs=wco[:, cc, half * 512:(half + 1) * 512], start=(cc == 0), stop=(cc == 7)),
                                   r=[("ocT", cc), ("wco", half * 2), ("wco", half * 2 + 1)], w=[("pf", bi)])
                        sch.op("dve", lambda e, s=s, xs=xs, half=half, bi=bi: e.tensor_tensor(
                            out=x2[:, xs, s, half * 512:(half + 1) * 512], in0=pf[bi][:, :],
                            in1=x2[:, xs, s, half * 512:(half + 1) * 512], op=ALU.add),
                               r=[("pf", bi), ("x2", xs, s)], w=[("x2", xs, s)])
                    sch.op("sp", lambda e, s=s, xs=xs, t=t: e.dma_start(out=C.x3[t * 128:(t + 1) * 128, :],
                                                                       in_=x2[:, xs, s, :]),
                           r=[("x2", xs, s)], w=[("x3", t)], dma="xs%d%d" % (xs, s))

            P1_norm(0)
            load_w()
            P1_wout(0)
            P1_norm2(0)
            P1_tr2(0)
            for ci in range(8):
                P2(ci)
                if ci + 1 < 8:
                    P1_norm(ci + 1)
                P3(ci)
                if ci + 1 < 8:
                    P1_wout(ci + 1)
                    P1_norm2(ci + 1)
                P4(ci)
                if ci + 1 < 8:
                    P1_tr2(ci + 1)
            sch.emit_stage()


def build_program(debug=False, upto=5):
    nc = bass.Bass("TRN2", target_bir_lowering=False)
    C = Ctx()

    def din(name, shape, dt=F32):
        return nc.dram_tensor(name, shape, dt, kind="ExternalInput")

    def dscr(name, shape, dt):
        return nc.dram_tensor(name, shape, dt, kind=("ExternalOutput" if debug else "Internal"))

    C.x = din("x", [S, D]).ap()
    C.mem = din("mem", [256, D]).ap()
    C.w_ffn1_gu = din("w_ffn1_gu", [D, 2 * DFF]).ap()
    C.w_ffn1_down = din("w_ffn1_down", [DFF, D]).ap()
    C.w_in = din("w_in", [D, 3 * D]).ap()
    C.w_out = din("w_out", [D, D]).ap()
    C.w_cq = din("w_cq", [D, D]).ap()
    C.w_ckv = din("w_ckv", [D, 2 * D]).ap()
    C.w_co = din("w_co", [D, D]).ap()
    C.w_ffn2_gu = din("w_ffn2_gu", [D, 2 * DFF]).ap()
    C.w_ffn2_down = din("w_ffn2_down", [DFF, D]).ap()
    C.gall_t = din("gall", [7, D])
    C.tb = din("tb", [4, 128, NW]).ap()
    C.cmask = din("cmask", [128, NW]).ap()
    C.tbs = din("tbs", [12, 128, 3, 256]).ap()
    C.cms = din("cms", [128, 256]).ap()
    C.kbind = din("kbind", [16, S]).ap()
    C.mconst = din("mconst", [128, 16, 16]).ap()
    C.identf = din("identf", [128, 128]).ap()
    C.out = nc.dram_tensor("out", [S, D], F32, kind="ExternalOutput").ap()
    C.x1 = dscr("x1", [S, D], F32).ap()
    C.qT = dscr("qT", [D, S], BF16).ap()
    C.kT = dscr("kT", [D, S], BF16).ap()
    C.vT = dscr("vT", [768, S], BF16).ap()
    C.V = dscr("V", [4, 128, NT, 65], BF16).ap()
    C.O = dscr("O", [NT, 128, D], F32).ap()
    C.x3 = dscr("x3", [S, D], F32).ap()

    with ExitStack() as es:
        C.ident = es.enter_context(nc.sbuf_tensor("ident", [128, 128], BF16))
        C.ones = es.enter_context(nc.sbuf_tensor("ones", [128, 128], BF16))
        C.mhalf = es.enter_context(nc.sbuf_tensor("mhalf", [128, 1], F32))
        C.st = es.enter_context(nc.sbuf_tensor("st", [128, 24], F32))
        sch = Sched(nc, es)
        sch.op("pool", lambda e: e.dma_start(out=C.ident[:, :], in_=C.identf), w=[("ident",)], dma="w0")
        sch.op("pool", lambda e: e.memset(C.ones[:, :], 1.0), w=[("ones",)])
        sch.op("pool", lambda e: e.memset(C.mhalf[:, :], -0.5), w=[("mhalf",)])
        sch.emit_stage()

        if upto >= 1:
            ffn_stage(nc, sch, C, "f1", C.x, C.x1, C.w_ffn1_gu, C.w_ffn1_down, 0, None)
        if upto >= 2:
            inproj_stage(nc, sch, C)
        if upto >= 3:
            attn_stage(nc, sch, C)
        if upto >= 4:
            cross_stage(nc, sch, C)
        if upto >= 5:
            ffn_stage(nc, sch, C, "f2", C.x3, C.out, C.w_ffn2_gu, C.w_ffn2_down, 5, 6)
        sch.final_wait()
    return nc


def _rel_bucket_np(dist):
    n = np.maximum(dist, 0)
    nf = np.maximum(n, 1).astype(np.float32)
    large = 16 + (np.log(nf / np.float32(16.0)) / np.float32(math.log(2048 / 16)) * np.float32(16)).astype(np.int32)
    large = np.minimum(large, 31)
    return np.where(n < 16, n, large)


def _host_consts(rel_bias):
    p = np.arange(128)[:, None]
    j = np.arange(NW)[None, :]
    Dm = j - WOFF - p
    bk = _rel_bucket_np(Dm)
    tb = np.ascontiguousarray(np.transpose(rel_bias[bk, 12:16], (2, 0, 1))).astype(np.float32)
    cmask = (Dm >= 0).astype(np.float32)
    c = np.arange(256)[None, :]
    dsub = np.maximum(c - p, 0)
    tbs = np.zeros((12, 128, 3, 256), np.float32)
    for di, d in enumerate((1, 4, 16)):
        bks = _rel_bucket_np(dsub * d)
        tbs[:, :, di, :] = np.transpose(rel_bias[bks, 0:12], (2, 0, 1))
    cms = (((c - p) >= 0) & ((c - p) <= 128)).astype(np.float32)
    kbind = np.zeros((16, S), np.float32)
    for jb in range(16):
        kbind[jb, jb * 256:(jb + 1) * 256] = 30000.0
    mconst = np.zeros((128, 16, 16), np.float32)
    for n in range(16):
        mconst[:, n, n] = 1e30
        mconst[:, n, n + 1:] = -1e30
    ident = np.eye(128, dtype=np.float32)
    return tb, cmask, tbs, cms, kbind, mconst, ident


_CACHE = {}


def _get_program(debug=False, upto=5):
    key = (debug, upto)
    if key not in _CACHE:
        _CACHE[key] = build_program(debug, upto)
    return _CACHE[key]


def _in_maps(inputs):
    f = lambda a: np.ascontiguousarray(np.asarray(a, dtype=np.float32))
    x = f(inputs["x"])
    mem = f(inputs["mem"])
    rel_bias = f(inputs["rel_bias"])
    tb, cmask, tbs, cms, kbind, mconst, ident = _host_consts(rel_bias)
    gall = np.stack([
        f(inputs["g_ffn1"])[0], f(inputs["g_mix"])[0],
        np.concatenate([f(inputs["g_out_a"])[0], f(inputs["g_out_b"])[0]]),
        f(inputs["g_cross"])[0], f(inputs["g_mem"])[0], f(inputs["g_ffn2"])[0], f(inputs["g_final"]),
    ]).astype(np.float32)
    shared = {
        "w_ffn1_gu": f(inputs["w_ffn1_gu"])[0], "w_ffn1_down": f(inputs["w_ffn1_down"])[0],
        "w_in": f(inputs["w_in"])[0], "w_out": f(inputs["w_out"])[0], "w_cq": f(inputs["w_cq"])[0],
        "w_ckv": f(inputs["w_ckv"])[0], "w_co": f(inputs["w_co"])[0],
        "w_ffn2_gu": f(inputs["w_ffn2_gu"])[0], "w_ffn2_down": f(inputs["w_ffn2_down"])[0],
        "gall": gall, "tb": tb, "cmask": cmask, "tbs": tbs, "cms": cms, "kbind": kbind, "mconst": mconst, "identf": ident,
    }
    maps = []
    for b in range(NCORES):
        m = dict(shared)
        m["x"] = np.ascontiguousarray(x[b])
        m["mem"] = np.ascontiguousarray(mem[b])
        maps.append(m)
    return maps


def kernel(**inputs):
    nc = _get_program(False, 5)
    maps = _in_maps(inputs)
    res = run_bass_kernel_spmd(nc, maps, core_ids=list(range(NCORES)))
    out = np.stack([np.asarray(r["out"], dtype=np.float32).reshape(S, D) for r in res.results], axis=0)
    return out
```

```python
import math
from contextlib import ExitStack

import numpy as np
import concourse.bass as bass
import concourse.mybir as mybir
from concourse.bass_utils import run_bass_kernel_spmd

F32 = mybir.dt.float32
BF16 = mybir.dt.bfloat16
AF = mybir.ActivationFunctionType
ALU = mybir.AluOpType
AX = mybir.AxisListType

S = 4096
D = 1024
DFF = 2816
NT = S // 128
NW = 2944
WOFF = 384
EPS = 1e-6
NCORES = 8


class Op:
    __slots__ = ("eng", "fn", "deps", "dma", "needs", "sem", "val")


class Sched:
    ENG = ("pe", "act", "dve", "pool", "sp")

    def __init__(self, nc, es):
        self.nc = nc
        self.es = es
        self.ops = {e: [] for e in self.ENG}
        self.tok = {}
        self.dma_last = {}
        self.dma_cnt = {}
        self.dma_sem = {}
        self.eng_sem = {}
        self.eng_cnt = {e: 0 for e in self.ENG}
        self.tail = {e: None for e in self.ENG}
        self.waited = {e: {} for e in self.ENG}
        self.barrier_deps = []
        for e in ("pe", "act", "dve", "pool"):
            self.eng_sem[e] = es.enter_context(nc.semaphore("sem_" + e))

    def _dsem(self, key):
        if key not in self.dma_sem:
            self.dma_sem[key] = self.es.enter_context(self.nc.semaphore("dsem_%d" % len(self.dma_sem)))
            self.dma_cnt[key] = 0
        return self.dma_sem[key]

    def op(self, eng, fn, r=(), w=(), dma=None):
        o = Op()
        o.eng, o.fn, o.dma, o.needs, o.deps = eng, fn, dma, False, []
        o.sem, o.val = None, 0
        deps = {}

        def add(d):
            if d is not None and d is not o:
                deps[id(d)] = d

        for t in r:
            st = self.tok.get(t)
            if st:
                add(st[0])
        for t in w:
            st = self.tok.get(t)
            if st:
                add(st[0])
                for rd in st[1].values():
                    add(rd)
                for rd in st[2]:
                    add(rd)
        if dma is not None:
            add(self.dma_last.get(dma))
            self.dma_last[dma] = o
            self._dsem(dma)
        for t in r:
            st = self.tok.setdefault(t, [None, {}, []])
            if dma is None:
                st[1][eng] = o
            else:
                st[2].append(o)
        for t in w:
            self.tok[t] = [o, {}, []]
        for d in deps.values():
            if d.dma is None and dma is None and d.eng == eng and eng == "pe":
                continue
            d.needs = True
            o.deps.append(d)
        self.ops[eng].append(o)
        if dma is None:
            self.tail[eng] = o
        return o

    def emit_stage(self):
        nc = self.nc
        for e in self.ENG:
            for o in self.ops[e]:
                if o.dma is not None:
                    self.dma_cnt[o.dma] += 1
                    o.sem = self.dma_sem[o.dma]
                    o.val = 16 * self.dma_cnt[o.dma]
                elif o.needs or o is self.tail[e]:
                    o.needs = True
                    self.eng_cnt[e] += 1
                    o.sem = self.eng_sem[e]
                    o.val = self.eng_cnt[e]
        bdeps = list(self.barrier_deps)

        def run(ename, eobj):
            waited = self.waited[ename]

            def wait_for(pairs):
                best = {}
                for sem, val in pairs:
                    k = id(sem)
                    if k not in best or best[k][1] < val:
                        best[k] = (sem, val)
                for k, (sem, val) in best.items():
                    if waited.get(k, 0) >= val:
                        continue
                    eobj.wait_ge(sem, val)
                    waited[k] = val

            wait_for(bdeps)
            for o in self.ops[ename]:
                wait_for([(d.sem, d.val) for d in o.deps])
                ins = o.fn(eobj)
                if o.dma is not None:
                    ins.then_inc(o.sem, 16)
                elif o.needs:
                    ins.then_inc(o.sem, 1)

        with nc.Block() as block:
            @block.tensor
            def _(e):
                run("pe", e)

            @block.scalar
            def _(e):
                run("act", e)

            @block.vector
            def _(e):
                run("dve", e)

            @block.gpsimd
            def _(e):
                run("pool", e)

            @block.sync
            def _(e):
                run("sp", e)

        nb = []
        for e in ("pe", "act", "dve", "pool"):
            if self.eng_cnt[e] > 0:
                nb.append((self.eng_sem[e], self.eng_cnt[e]))
        for k, sem in self.dma_sem.items():
            if self.dma_cnt[k] > 0:
                nb.append((sem, 16 * self.dma_cnt[k]))
        self.barrier_deps = nb
        self.ops = {e: [] for e in self.ENG}
        self.tok = {}
        self.dma_last = {}
        self.tail = {e: None for e in self.ENG}

    def final_wait(self):
        nc = self.nc
        bdeps = list(self.barrier_deps)
        with nc.Block() as block:
            @block.sync
            def _(e):
                for sem, val in bdeps:
                    e.wait_ge(sem, val)

            @block.gpsimd
            def _(e):
                for sem, val in bdeps:
                    e.wait_ge(sem, val)


class Ctx:
    pass


_PSN = [0]


def _psum_banks(nc, es, n_f32, n_bf16):
    _PSN[0] += 1
    u = _PSN[0]
    pf = [es.enter_context(nc.psum_tensor("pf%d_%d" % (u, i), [128, 512], F32)) for i in range(n_f32)]
    pb = [es.enter_context(nc.psum_tensor("pb%d_%d" % (u, i), [128, 1024], BF16)) for i in range(n_bf16)]
    return pf, pb


def _load_gain(sch, C, dst, row, key):
    src = bass.AP(C.gall_t, row * D, [[0, 128], [1, D]])
    sch.op("sp", lambda e: e.dma_start(out=dst[:, :], in_=src), w=[("g", key)], dma="g" + key)


def _norm_to_hn(sch, C, xap, xtok, gB, gkey, hn_ap, hntok, k):
    st = C.st
    b = (k % 4) * 3
    sa, sb, sc = st[:, b:b + 1], st[:, b + 1:b + 2], st[:, b + 2:b + 3]
    tk = ("st", k % 4)
    sch.op("act", lambda e: e.activation(out=hn_ap, in_=xap, func=AF.Square, accum_out=sa),
           r=[xtok], w=[hntok, tk])
    sch.op("dve", lambda e: e.tensor_scalar(out=sb, in0=sa, scalar1=1.0 / D, scalar2=EPS,
                                            op0=ALU.mult, op1=ALU.add), r=[tk], w=[tk])
    sch.op("pool", lambda e: e.tensor_tensor(out=sc, in0=sb, in1=C.mhalf[:, 0:1], op=ALU.pow),
           r=[tk, ("mhalf",)], w=[tk])
    sch.op("dve", lambda e: e.scalar_tensor_tensor(out=hn_ap, in0=xap, scalar=sc, in1=gB[:, :],
                                                   op0=ALU.mult, op1=ALU.mult),
           r=[xtok, tk, ("g", gkey)], w=[hntok])


def _transpose_to(sch, C, hn_ap_fn, hntok, pb, pbi, dstT, dsttok, col0, evac_eng="act"):
    ps = pb[pbi]
    ptok = ("pb", pbi)
    for cc in range(8):
        sch.op("pe", lambda e, cc=cc: e.transpose(out=ps[:, cc * 128:(cc + 1) * 128],
                                                  in_=hn_ap_fn(cc), identity=C.ident[:, :]),
               r=[hntok, ("ident",)], w=[ptok])
    src = ps[:, :].rearrange("p (c t) -> p c t", c=8)
    dst = dstT[:, :, col0:col0 + 128]
    if evac_eng == "act":
        sch.op("act", lambda e: e.copy(out=dst, in_=src), r=[ptok], w=[dsttok])
    else:
        sch.op("dve", lambda e: e.tensor_copy(out=dst, in_=src), r=[ptok], w=[dsttok])


def _load_weight_cols(sch, dst3, src2, c0, c1, tok, keyi, dst_c0=None):
    if dst_c0 is None:
        dst_c0 = c0
    srcv = src2.rearrange("(kc p) n -> p kc n", p=128)[:, :, c0:c1]
    dstv = dst3[:, :, dst_c0:dst_c0 + (c1 - c0)]
    sch.op("pool", lambda e: e.dma_start(out=dstv, in_=srcv), w=[tok], dma="w%d" % (keyi % 8))


def ffn_stage(nc, sch, C, name, xin, xout, wgu_d, wdn_d, grow, frow):
    with ExitStack() as es:
        T = lambda n, shp, dt: es.enter_context(nc.sbuf_tensor(name + n, shp, dt))
        wgu = T("wgu", [128, 8, 2 * DFF], BF16)
        wdn = T("wdn", [128, 22, D], BF16)
        gB = T("gB", [128, D], F32)
        gF = T("gF", [128, D], F32) if frow is not None else None
        xt = T("xt", [128, 4, D], F32)
        hn = T("hn", [128, 4, D], BF16)
        hT = T("hT", [128, 8, 512], BF16)
        aT = T("aT", [128, 22, 512], BF16)
        sg = T("sg", [128, 2, 512], F32)
        pf, pb = _psum_banks(nc, es, 6, 2)
        psA, psB, psC = pf[0:2], pf[2:4], pf[4:6]

        _load_gain(sch, C, gB, grow, "B")
        if frow is not None:
            _load_gain(sch, C, gF, frow, "F")
        def load_weights():
            ki = 0
            for j in range(4):
                _load_weight_cols(sch, wgu, wgu_d, j * 704, (j + 1) * 704, ("wgu", j), ki); ki += 1
                _load_weight_cols(sch, wgu, wgu_d, DFF + j * 704, DFF + (j + 1) * 704, ("wgu", j), ki); ki += 1
            for j in range(11):
                srcv = wdn_d.rearrange("(fc p) n -> p fc n", p=128)[:, 2 * j:2 * j + 2, :]
                dstv = wdn[:, 2 * j:2 * j + 2, :]
                sch.op("pool", lambda e, dstv=dstv, srcv=srcv: e.dma_start(out=dstv, in_=srcv),
                       w=[("wdn", j)], dma="w%d" % (ki % 8)); ki += 1

        cnt = {"x": 0, "k": 0}

        def load_x(src_d, t):
            slot = cnt["x"] % 4
            cnt["x"] += 1
            sch.op("sp", lambda e: e.dma_start(out=xt[:, slot, :], in_=src_d[t * 128:(t + 1) * 128, :]),
                   w=[("xt", slot)], dma="xl%d" % slot)
            return slot

        def phaseA_norm(ci):
            for s in range(4):
                t = ci * 4 + s
                slot = load_x(xin, t)
                k = ci * 4 + s
                hnap = hn[:, k % 4, :]
                _norm_to_hn(sch, C, xt[:, slot, :], ("xt", slot), gB, "B", hnap, ("hn", k % 4), k)

        def phaseA_tr(ci):
            for s in range(4):
                k = ci * 4 + s
                _transpose_to(sch, C, lambda cc, k=k: hn[:, k % 4, cc * 128:(cc + 1) * 128], ("hn", k % 4),
                              pb, k % 2, hT, ("hT",), s * 128)

        def phaseB(ci):
            for fi in range(22):
                pa, pbk = psA[fi % 2], psB[fi % 2]
                ta, tb_ = ("pf", fi % 2), ("pf", 2 + fi % 2)
                for cc in range(8):
                    sch.op("pe", lambda e, cc=cc, pa=pa, fi=fi: e.matmul(
                        pa[:, :], lhsT=wgu[:, cc, fi * 128:(fi + 1) * 128], rhs=hT[:, cc, :],
                        start=(cc == 0), stop=(cc == 7)),
                           r=[("wgu", (fi * 128) // 704), ("wgu", (fi * 128 + 127) // 704), ("hT",)], w=[ta])
                for cc in range(8):
                    sch.op("pe", lambda e, cc=cc, pbk=pbk, fi=fi: e.matmul(
                        pbk[:, :], lhsT=wgu[:, cc, DFF + fi * 128:DFF + (fi + 1) * 128], rhs=hT[:, cc, :],
                        start=(cc == 0), stop=(cc == 7)),
                           r=[("wgu", (fi * 128) // 704), ("wgu", (fi * 128 + 127) // 704), ("hT",)], w=[tb_])
                sgap = sg[:, fi % 2, :]
                sch.op("act", lambda e, pa=pa, sgap=sgap: e.activation(out=sgap, in_=pa[:, :], func=AF.Silu),
                       r=[ta], w=[("sg", fi % 2)])
                sch.op("dve", lambda e, pbk=pbk, sgap=sgap, fi=fi: e.tensor_tensor(
                    out=aT[:, fi, :], in0=sgap, in1=pbk[:, :], op=ALU.mult),
                       r=[("sg", fi % 2), tb_], w=[("aT", fi)])

        def phaseC(ci):
            for s in range(4):
                t = ci * 4 + s
                slot = load_x(xin, t)
                xr = xt[:, slot, :]
                for half in range(2):
                    pc = psC[half]
                    tc_ = ("pf", 4 + half)
                    for fi in range(22):
                        sch.op("pe", lambda e, fi=fi, pc=pc, half=half, s=s: e.matmul(
                            pc[:, :], lhsT=aT[:, fi, s * 128:(s + 1) * 128],
                            rhs=wdn[:, fi, half * 512:(half + 1) * 512],
                            start=(fi == 0), stop=(fi == 21)), r=[("aT", fi), ("wdn", fi // 2)], w=[tc_])
                    xh = xt[:, slot, half * 512:(half + 1) * 512]
                    sch.op("dve", lambda e, pc=pc, xh=xh: e.scalar_tensor_tensor(
                        out=xh, in0=pc[:, :], scalar=0.5, in1=xh, op0=ALU.mult, op1=ALU.add),
                           r=[tc_, ("xt", slot)], w=[("xt", slot)])
                if frow is not None:
                    k = cnt["k"]; cnt["k"] += 1
                    b = (k % 4) * 3
                    sa, sb, sc = C.st[:, b:b + 1], C.st[:, b + 1:b + 2], C.st[:, b + 2:b + 3]
                    tk = ("st", k % 4)
                    junk = sg[:, :, :].rearrange("p a b -> p (a b)")
                    sch.op("act", lambda e, xr=xr, sa=sa, junk=junk: e.activation(
                        out=junk, in_=xr, func=AF.Square, accum_out=sa),
                           r=[("xt", slot)], w=[("sg", 0), ("sg", 1), tk])
                    sch.op("dve", lambda e, sa=sa, sb=sb: e.tensor_scalar(
                        out=sb, in0=sa, scalar1=1.0 / D, scalar2=EPS, op0=ALU.mult, op1=ALU.add), r=[tk], w=[tk])
                    sch.op("pool", lambda e, sb=sb, sc=sc: e.tensor_tensor(
                        out=sc, in0=sb, in1=C.mhalf[:, 0:1], op=ALU.pow), r=[tk, ("mhalf",)], w=[tk])
                    sch.op("dve", lambda e, xr=xr, sc=sc: e.scalar_tensor_tensor(
                        out=xr, in0=xr, scalar=sc, in1=gF[:, :], op0=ALU.mult, op1=ALU.mult),
                           r=[("xt", slot), tk, ("g", "F")], w=[("xt", slot)])
                sch.op("sp", lambda e, xr=xr, t=t: e.dma_start(out=xout[t * 128:(t + 1) * 128, :], in_=xr),
                       r=[("xt", slot)], w=[("xout", t)], dma="xs%d" % slot)

        cnt["k"] = 64
        phaseA_norm(0)
        load_weights()
        phaseA_tr(0)
        if True:
            phaseA_norm(1)
        phaseB(0)
        for ci in range(8):
            if ci + 1 < 8:
                phaseA_tr(ci + 1)
            phaseC(ci)
            if ci + 2 < 8:
                phaseA_norm(ci + 2)
            if ci + 1 < 8:
                phaseB(ci + 1)
        sch.emit_stage()


def inproj_stage(nc, sch, C):
    with ExitStack() as es:
        T = lambda n, shp, dt: es.enter_context(nc.sbuf_tensor("ip" + n, shp, dt))
        win = T("win", [128, 8, 3072], BF16)
        gB = T("gB", [128, D], F32)
        xt = T("xt", [128, 4, D], F32)
        hn = T("hn", [128, 4, D], BF16)
        hT = T("hT", [128, 8, 512], BF16)
        qk = T("qk", [128, 2, 22, 512], BF16)
        vt = T("vt", [128, 2, 4, 4, 65], BF16)
        pf, pb = _psum_banks(nc, es, 6, 2)

        _load_gain(sch, C, gB, 1, "B")
        sch.op("pool", lambda e: e.memset(vt[:, :, :, :, 64:65], 1.0), w=[("vt", 0), ("vt", 1)])

        fcs = [0, 1, 2, 3, 4, 5, 18, 19, 6, 7, 8, 9, 10, 11, 20, 21, 12, 13, 14, 15, 16, 17]
        qTv = C.qT.rearrange("(i p) t -> p i t", p=128)
        kTv = C.kT.rearrange("(i p) t -> p i t", p=128)
        vTv = C.vT.rearrange("(i p) t -> p i t", p=128)
        Vv = C.V.rearrange("h p t e -> p h t e")
        cnt = {"x": 0, "k": 0, "pf": 0}

        def phaseA_norm(ci):
            for s in range(4):
                t = ci * 4 + s
                slot = cnt["x"] % 4; cnt["x"] += 1
                sch.op("sp", lambda e, slot=slot, t=t: e.dma_start(out=xt[:, slot, :],
                                                                  in_=C.x1[t * 128:(t + 1) * 128, :]),
                       w=[("xt", slot)], dma="xl%d" % slot)
                k = ci * 4 + s
                _norm_to_hn(sch, C, xt[:, slot, :], ("xt", slot), gB, "B", hn[:, k % 4, :], ("hn", k % 4), k)

        def phaseA_tr(ci):
            for s in range(4):
                k = ci * 4 + s
                _transpose_to(sch, C, lambda cc, k=k: hn[:, k % 4, cc * 128:(cc + 1) * 128], ("hn", k % 4),
                              pb, k % 2, hT, ("hT",), s * 128)

        def phaseB(ci):
            sl = ci % 2
            for i, fc in enumerate(fcs):
                bi = cnt["pf"] % 6; cnt["pf"] += 1
                ps = pf[bi]
                for cc in range(8):
                    sch.op("pe", lambda e, cc=cc, ps=ps, fc=fc: e.matmul(
                        ps[:, :], lhsT=win[:, cc, fc * 128:(fc + 1) * 128], rhs=hT[:, cc, :],
                        start=(cc == 0), stop=(cc == 7)), r=[("win", fc // 2), ("hT",)], w=[("pf", bi)])
                dst = qk[:, sl, i, :]
                if i % 2 == 0:
                    sch.op("act", lambda e, ps=ps, dst=dst: e.copy(out=dst, in_=ps[:, :]),
                           r=[("pf", bi)], w=[("qk", sl, i)])
                else:
                    sch.op("dve", lambda e, ps=ps, dst=dst: e.tensor_copy(out=dst, in_=ps[:, :]),
                           r=[("pf", bi)], w=[("qk", sl, i)])
            for (dv, base, lo, n, nm) in ((qTv, 0, 0, 4, "q0"), (qTv, 0, 4, 4, "q1"), (kTv, 8, 0, 4, "k0"),
                                          (kTv, 8, 4, 4, "k1"), (vTv, 16, 0, 3, "v0"), (vTv, 16, 3, 3, "v1")):
                sch.op("sp", lambda e, sl=sl, ci=ci, dv=dv, base=base, lo=lo, n=n: e.dma_start(
                    out=dv[:, lo:lo + n, ci * 512:(ci + 1) * 512], in_=qk[:, sl, base + lo:base + lo + n, :]),
                       r=[("qk", sl, i) for i in range(base + lo, base + lo + n)], w=[("st" + nm, ci)],
                       dma="qs%s%d" % (nm, sl))
            for s in range(4):
                bi = cnt["pf"] % 6; cnt["pf"] += 1
                ps = pf[bi]
                for cc in range(8):
                    sch.op("pe", lambda e, cc=cc, ps=ps, s=s: e.matmul(
                        ps[:, 0:256], lhsT=hT[:, cc, s * 128:(s + 1) * 128], rhs=win[:, cc, 2816:3072],
                        start=(cc == 0), stop=(cc == 7)),
                           r=[("win", 11), ("hT",)], w=[("pf", bi)])
                src = ps[:, 0:256].rearrange("p (h e) -> p h e", e=64)
                dst = vt[:, sl, :, s, 0:64]
                sch.op("dve", lambda e, src=src, dst=dst: e.tensor_copy(out=dst, in_=src),
                       r=[("pf", bi)], w=[("vtw", sl, s)])
            sch.op("sp", lambda e, sl=sl, ci=ci: e.dma_start(
                out=Vv[:, :, ci * 4:(ci + 1) * 4, :], in_=vt[:, sl, :, :, :]),
                   r=[("vt", sl)] + [("vtw", sl, s) for s in range(4)],
                   w=[("V", ci)], dma="vs%d" % sl)

        phaseA_norm(0)
        for j in range(12):
            _load_weight_cols(sch, win, C.w_in, j * 256, (j + 1) * 256, ("win", j), j)
        phaseA_tr(0)
        for ci in range(8):
            if ci + 1 < 8:
                phaseA_norm(ci + 1)
            phaseB(ci)
            if ci + 1 < 8:
                phaseA_tr(ci + 1)
        sch.emit_stage()


def attn_stage(nc, sch, C, heads=range(16)):
    with ExitStack() as es:
        T = lambda n, shp, dt: es.enter_context(nc.sbuf_tensor("at" + n, shp, dt))
        qt = T("qt", [128, 2, S], BF16)
        kt = T("kt", [128, 2, S], BF16)
        vtT = T("vtT", [128, 2, S], BF16)
        vt = T("vt", [128, 2, 32, 65], BF16)
        tbs = T("tbs", [128, 2, 3, 256], F32)
        cms = T("cms", [128, 256], F32)
        Ws = T("Ws", [128, 2, 3, 256], BF16)
        vk = T("vk", [128, 3, 65], BF16)
        acc = T("acc", [128, S], F32)
        tbm = T("tbm", [128, 2, NW], F32)
        cm = T("cm", [128, NW], F32)
        Wm = T("Wm", [128, 2, NW], BF16)
        Eb = T("Eb", [128, 3, 512], BF16)
        Pb = T("Pb", [128, 3, 512], BF16)
        oh = T("oh", [128, 2, 32, 64], F32)
        km = T("km", [128, 80], BF16)
        kmf = T("kmf", [128, 16], F32)
        gmf = T("gmf", [128, 4, 16], F32)
        m8 = T("m8", [128, 4, 8], F32)
        selin = T("selin", [128, 4, 80], F32)
        mcon = T("mcon", [128, 16, 16], F32)
        rd = T("rd", [128, 8], F32)
        idf = T("idf", [128, 128], F32)
        psS = [es.enter_context(nc.psum_tensor("psS%d" % i, [128, 512], F32)) for i in range(2)]
        psO = [es.enter_context(nc.psum_tensor("psO%d" % i, [128, 512], F32)) for i in range(4)]
        psM = es.enter_context(nc.psum_tensor("psM", [128, 512], F32))
        psVb = es.enter_context(nc.psum_tensor("psVb", [128, 1024], BF16))
        psG = psM

        sch.op("sp", lambda e: e.dma_start(out=idf[:, :], in_=C.identf), w=[("idf",)], dma="c2")
        sch.op("sp", lambda e: e.dma_start(out=cm[:, :], in_=C.cmask), w=[("cm",)], dma="c0")
        sch.op("sp", lambda e: e.dma_start(out=cms[:, :], in_=C.cms), w=[("cms",)], dma="c3")
        sch.op("sp", lambda e: e.dma_start(out=mcon[:, :, :], in_=C.mconst), w=[("mcon",)], dma="c1")
        for sl in range(2):
            sch.op("pool", lambda e, sl=sl: e.memset(kt[64:128, sl, :], 0.0), w=[("ktaug", sl)])
            sch.op("pool", lambda e, sl=sl: e.memset(qt[64:128, sl, :], 0.0), w=[("qtz", sl)])
            sch.op("pool", lambda e, sl=sl: e.memset(vtT[64:128, sl, :], 0.0), w=[("vtz", sl)])
            sch.op("pool", lambda e, sl=sl: e.dma_start(
                out=kt[64:80, sl, :].rearrange("j (a b) -> j a b", b=2048),
                in_=C.kbind.rearrange("j (a b) -> j a b", b=2048)),
                   w=[("ktaug", sl)], dma="w%d" % sl)
        sch.op("pool", lambda e: e.memset(km[:, :], 0.0), w=[("km",)])
        sch.op("pool", lambda e: e.memset(selin[:, :, :], 0.0), w=[("selin", 0), ("selin", 1), ("selin", 2), ("selin", 3)])
        sch.op("pool", lambda e: e.memset(vk[:, :, 64:65], 1.0), w=[("vk", 0), ("vk", 1), ("vk", 2)])
        Ov = C.O.rearrange("t p f -> p t f")

        def load_head(h):
            sl = h % 2
            sch.op("sp", lambda e: e.dma_start(out=qt[0:64, sl, :], in_=C.qT[h * 64:(h + 1) * 64, :]),
                   w=[("qt", sl)], dma="hq%d" % sl)
            sch.op("sp", lambda e: e.dma_start(out=kt[0:64, sl, :], in_=C.kT[h * 64:(h + 1) * 64, :]),
                   w=[("kt", sl)], dma="hk%d" % sl)
            if h < 12:
                sch.op("sp", lambda e: e.dma_start(out=vtT[0:64, sl, :], in_=C.vT[h * 64:(h + 1) * 64, :]),
                       w=[("vtT", sl)], dma="hv%d" % sl)
                sch.op("sp", lambda e: e.dma_start(out=tbs[:, sl, :, :], in_=C.tbs[h]),
                       w=[("tbs", sl)], dma="ht%d" % sl)
            else:
                sch.op("sp", lambda e: e.dma_start(out=vt[:, sl, :, :], in_=C.V[h - 12]),
                       w=[("vth", sl)], dma="hv%d" % sl)
                sch.op("sp", lambda e: e.dma_start(out=tbm[:, sl, :], in_=C.tb[h - 12]),
                       w=[("tbm", sl)], dma="ht%d" % sl)

        def dilated_head(h, sl):
            sch.op("act", lambda e: e.activation(out=tbs[:, sl, :, :], in_=tbs[:, sl, :, :], func=AF.Exp),
                   r=[("tbs", sl)], w=[("tbs", sl)])
            for di in range(3):
                sch.op("dve", lambda e, di=di: e.tensor_tensor(out=Ws[:, sl, di, :], in0=tbs[:, sl, di, :],
                                                             in1=cms[:, :], op=ALU.mult),
                       r=[("tbs", sl), ("cms",)], w=[("Ws", sl)])
            items = []
            for di, d in enumerate((1, 4, 16)):
                nt_ = (S // d) // 128
                for r in range(d):
                    for jt in range(nt_):
                        items.append((di, d, r, jt, nt_))
            n = len(items)

            psS3 = [psS[0], psS[1], psO[3]]
            psS3tok = [("psS", 0), ("psS", 1), ("psO", 3)]

            def front(i):
                di, d, r, jt, nt_ = items[i]
                b = i % 3
                b3 = i % 3
                nq = 256 if jt + 1 < nt_ else 128
                ks = jt * 128 * d + r
                lhs = kt[0:128, sl, ks:ks + 127 * d + 1:d]
                rhs = qt[0:128, sl, ks:ks + (nq - 1) * d + 1:d]
                pS = psS3[b]
                sch.op("pe", lambda e, lhs=lhs, rhs=rhs, pS=pS, nq=nq: e.matmul(
                    pS[:, 0:nq], lhsT=lhs, rhs=rhs, start=True, stop=True),
                       r=[("kt", sl), ("ktaug", sl), ("qt", sl), ("qtz", sl)], w=[psS3tok[b]])
                eb = Eb[:, b3, 0:nq]
                sch.op("act", lambda e, eb=eb, pS=pS, nq=nq: e.activation(out=eb, in_=pS[:, 0:nq], func=AF.Exp,
                                                                       scale=0.125),
                       r=[psS3tok[b]], w=[("Eb", b3)])
                pbv = Pb[:, b3, 0:nq]
                wv = Ws[:, sl, di, 0:nq]
                sch.op("pool" if i % 2 == 1 else "dve",
                       lambda e, pbv=pbv, eb=eb, wv=wv: e.tensor_tensor(out=pbv, in0=eb, in1=wv, op=ALU.mult),
                       r=[("Eb", b3), ("Ws", sl)], w=[("Pb", b3)])
                vin = vtT[0:128, sl, ks:ks + 127 * d + 1:d]
                sch.op("pe", lambda e, vin=vin: e.transpose(out=psVb[:, 0:128], in_=vin, identity=C.ident[:, :]),
                       r=[("vtT", sl), ("vtz", sl), ("ident",)], w=[("psVb",)])
                vkv = vk[:, b3, 0:64]
                sch.op("act", lambda e, vkv=vkv: e.copy(out=vkv, in_=psVb[:, 0:64]),
                       r=[("psVb",)], w=[("vk", b3)])

            def back(i):
                di, d, r, jt, nt_ = items[i]
                b3 = i % 3
                for qi in range(2):
                    jq = jt + qi
                    if jq >= nt_:
                        continue
                    bank = jq % 2
                    po = psO[bank][0:65, 0:128]
                    lhs = vk[:, b3, 0:65]
                    rhs = Pb[:, b3, qi * 128:(qi + 1) * 128]
                    st_ = (qi == 1) or (jq == 0)
                    sp_ = (qi == 0)
                    sch.op("pe", lambda e, po=po, lhs=lhs, rhs=rhs, st_=st_, sp_=sp_: e.matmul(
                        po, lhsT=lhs, rhs=rhs, start=st_, stop=sp_),
                           r=[("vk", b3), ("Pb", b3)], w=[("psO", bank)])
                    if sp_:
                        qs = jq * 128 * d + r
                        av = acc[0:65, qs:qs + 127 * d + 1:d]
                        if di == 0:
                            sch.op("act", lambda e, av=av, po=po: e.copy(out=av, in_=po),
                                   r=[("psO", bank)], w=[("acc",)])
                        else:
                            sch.op("dve", lambda e, av=av, po=po: e.tensor_tensor(out=av, in0=av, in1=po, op=ALU.add),
                                   r=[("psO", bank), ("acc",)], w=[("acc",)])

            for i in range(n + 2):
                if i < n:
                    front(i)
                if i >= 2:
                    back(i - 2)
            for t in range(NT):
                s4 = t % 4
                pF = psO[2 + t % 2]
                sch.op("pe", lambda e, t=t, pF=pF: e.transpose(out=pF[:, 0:65], in_=acc[0:65, t * 128:(t + 1) * 128],
                                                           identity=idf[0:65, 0:65]),
                       r=[("acc",), ("idf",)], w=[("psO", 2 + t % 2)])
                sch.op("dve", lambda e, s4=s4, pF=pF: e.reciprocal(out=rd[:, s4:s4 + 1], in_=pF[:, 64:65]),
                       r=[("psO", 2 + t % 2)], w=[("rd", s4)])
                sch.op("dve", lambda e, s4=s4, pF=pF, t=t: e.tensor_scalar(
                    out=oh[:, sl, t, :], in0=pF[:, 0:64], scalar1=rd[:, s4:s4 + 1], scalar2=None, op0=ALU.mult),
                       r=[("psO", 2 + t % 2), ("rd", s4)], w=[("oh", sl, t)])
                if s4 == 3:
                    qc = t // 4
                    sch.op("sp", lambda e, qc=qc: e.dma_start(
                        out=Ov[:, qc * 4:(qc + 1) * 4, h * 64:(h + 1) * 64], in_=oh[:, sl, qc * 4:(qc + 1) * 4, :]),
                           r=[("oh", sl, qc * 4 + s) for s in range(4)], w=[("O", h, qc)], dma="os%d" % (qc % 4))

        def moba_head(h, sl):
            K = 128
            sch.op("act", lambda e: e.activation(out=tbm[:, sl, :], in_=tbm[:, sl, :], func=AF.Exp),
                   r=[("tbm", sl)], w=[("tbm", sl)])
            sch.op("dve", lambda e: e.tensor_tensor(out=Wm[:, sl, :], in0=tbm[:, sl, :], in1=cm[:, :], op=ALU.mult),
                   r=[("tbm", sl), ("cm",)], w=[("Wm", sl)])
            ktok = [("kt", sl), ("ktaug", sl)]
            sch.op("dve", lambda e: e.tensor_reduce(
                out=kmf[0:64, :], in_=kt[0:64, sl, :].rearrange("p (a b) -> p a b", b=256), axis=AX.X, op=ALU.add),
                   r=[("kt", sl)], w=[("kmf",)])
            sch.op("dve", lambda e: e.tensor_copy(out=km[0:64, 64:80], in_=kmf[0:64, :]),
                   r=[("kmf",)], w=[("km",)])
            gbanks = [psS[0], psS[1], psO[0], psO[1], psO[2], psO[3]]
            gtoks = [("psS", 0), ("psS", 1), ("psO", 0), ("psO", 1), ("psO", 2), ("psO", 3)]

            def gate_front(t):
                n = t // 2
                g = t % 4
                bk, bt = gbanks[t % 6], gtoks[t % 6]
                sch.op("pe", lambda e: e.matmul(
                    bk[:, 0:80], lhsT=qt[0:128, sl, t * 128:(t + 1) * 128], rhs=km[0:128, 0:80],
                    start=True, stop=True), r=[("qt", sl), ("qtz", sl), ("km",)], w=[bt])
                sch.op("dve", lambda e: e.tensor_tensor(
                    out=gmf[:, g, :], in0=bk[:, 64:80], in1=mcon[:, n, :], op=ALU.add),
                       r=[bt, ("mcon",)], w=[("gmf", g)])
                sch.op("dve", lambda e: e.max(out=m8[:, g, :], in_=gmf[:, g, :]),
                       r=[("gmf", g)], w=[("m8", g)])
                sch.op("dve", lambda e: e.tensor_scalar(
                    out=selin[:, g, 64:80], in0=gmf[:, g, :], scalar1=m8[:, g, 3:4], scalar2=1.0,
                    op0=ALU.is_ge, op1=ALU.subtract), r=[("gmf", g), ("m8", g)], w=[("selin", g)])

            def gate_back(t):
                g = t % 4
                bk, bt = gbanks[t % 6], gtoks[t % 6]
                sch.op("pe", lambda e: e.transpose(out=bk[0:80, 128:256], in_=selin[:, g, 0:80],
                                                   identity=idf[:, :]),
                       r=[("selin", g), ("idf",)], w=[bt])
                sch.op("act", lambda e: e.copy(out=qt[64:80, sl, t * 128:(t + 1) * 128],
                                               in_=bk[64:80, 128:256]),
                       r=[bt], w=[("qtaug", sl, t)])

            for t in range(NT + 2):
                if t < NT:
                    gate_front(t)
                if t >= 2:
                    gate_back(t - 2)
            for qc in range(8):
                ktl = list(range(0, qc * 4 + 4))
                nk = len(ktl)

                def valid(kti, s, qc=qc):
                    return kti * 128 <= qc * 512 + s * 128 + 127

                firsts = {s: min(k for k in ktl if valid(k, s)) for s in range(4)}
                lasts = {s: max(k for k in ktl if valid(k, s)) for s in range(4)}
                qr = [("qt", sl), ("qtz", sl)] + [("qtaug", sl, qc * 4 + s) for s in range(4)]

                mS3 = [psS[0], psS[1], psM]
                mS3tok = [("psS", 0), ("psS", 1), ("psM",)]

                def QK(i, qc=qc, ktl=ktl, qr=qr):
                    kti = ktl[i]
                    b3 = i % 3
                    lhs = kt[0:K, sl, kti * 128:(kti + 1) * 128]
                    rhs = qt[0:K, sl, qc * 512:(qc + 1) * 512]
                    pS = mS3[b3]
                    sch.op("pe", lambda e, lhs=lhs, rhs=rhs, pS=pS: e.matmul(
                        pS[:, :], lhsT=lhs, rhs=rhs, start=True, stop=True),
                           r=ktok + qr, w=[mS3tok[b3]])
                    eb = Eb[:, b3, :]
                    sch.op("act", lambda e, eb=eb, pS=pS: e.activation(out=eb, in_=pS[:, :], func=AF.Exp, scale=0.125),
                           r=[mS3tok[b3]], w=[("Eb", b3)])
                    off = min(qc * 512 - kti * 128, 2048) + WOFF
                    pbv = Pb[:, b3, :]
                    wv = Wm[:, sl, off:off + 512]
                    sch.op("pool" if i % 3 == 1 else "dve",
                           lambda e, pbv=pbv, eb=eb, wv=wv: e.tensor_tensor(out=pbv, in0=eb, in1=wv, op=ALU.mult),
                           r=[("Eb", b3), ("Wm", sl)], w=[("Pb", b3)])

                def PV(i, ktl=ktl, firsts=firsts, lasts=lasts, valid=valid):
                    kti = ktl[i]
                    b3 = i % 3
                    for s in range(4):
                        if not valid(kti, s):
                            continue
                        po = psO[s][:, 0:65]
                        lhs = Pb[:, b3, s * 128:(s + 1) * 128]
                        rhs = vt[:, sl, kti, :]
                        st_ = (kti == firsts[s])
                        sp_ = (kti == lasts[s])
                        sch.op("pe", lambda e, po=po, lhs=lhs, rhs=rhs, st_=st_, sp_=sp_: e.matmul(
                            po, lhsT=lhs, rhs=rhs, start=st_, stop=sp_),
                               r=[("Pb", b3), ("vth", sl)], w=[("psO", s)])

                for i in range(nk + 2):
                    if i < nk:
                        QK(i)
                    if i >= 2:
                        PV(i - 2)
                for s in range(4):
                    t = qc * 4 + s
                    sch.op("dve", lambda e, s=s: e.reciprocal(out=rd[:, 4 + s:5 + s], in_=psO[s][:, 64:65]),
                           r=[("psO", s)], w=[("rd", 4 + s)])
                    sch.op("dve", lambda e, s=s, t=t: e.tensor_scalar(
                        out=oh[:, sl, t, :], in0=psO[s][:, 0:64], scalar1=rd[:, 4 + s:5 + s], scalar2=None,
                        op0=ALU.mult), r=[("psO", s), ("rd", 4 + s)], w=[("oh", sl, t)])
                sch.op("sp", lambda e, qc=qc: e.dma_start(
                    out=Ov[:, qc * 4:(qc + 1) * 4, h * 64:(h + 1) * 64], in_=oh[:, sl, qc * 4:(qc + 1) * 4, :]),
                       r=[("oh", sl, qc * 4 + s) for s in range(4)], w=[("O", h, qc)], dma="os%d" % (qc % 4))

        hl = list(heads)
        load_head(hl[0])
        for hi, h in enumerate(hl):
            sl = h % 2
            if hi + 1 < len(hl):
                load_head(hl[hi + 1])
            if h < 12:
                dilated_head(h, sl)
            else:
                moba_head(h, sl)
        sch.emit_stage()


def cross_stage(nc, sch, C):
    with ExitStack() as es0:
        T0 = lambda n, shp, dt: es0.enter_context(nc.sbuf_tensor("cr" + n, shp, dt))
        kcT = T0("kcT", [128, 8, 256], BF16)
        vc = T0("vc", [128, 2, D], BF16)
        with ExitStack() as es:
            T = lambda n, shp, dt: es.enter_context(nc.sbuf_tensor("cm" + n, shp, dt))
            wckv = T("wckv", [128, 8, 2 * D], BF16)
            gM = T("gM", [128, D], F32)
            mt_ = T("mt", [128, 2, D], F32)
            hn = T("hn", [128, 2, D], BF16)
            hT = T("hT", [128, 8, 256], BF16)
            pf, pb = _psum_banks(nc, es, 6, 2)
            _load_gain(sch, C, gM, 4, "M")
            for mt in range(2):
                sch.op("sp", lambda e, mt=mt: e.dma_start(out=mt_[:, mt, :], in_=C.mem[mt * 128:(mt + 1) * 128, :]),
                       w=[("mt", mt)], dma="ol%d" % mt)
                _norm_to_hn(sch, C, mt_[:, mt, :], ("mt", mt), gM, "M", hn[:, mt, :], ("hn", mt), mt)
            for j in range(8):
                _load_weight_cols(sch, wckv, C.w_ckv, j * 256, (j + 1) * 256, ("wckv", j), j)
            for mt in range(2):
                _transpose_to(sch, C, lambda cc, mt=mt: hn[:, mt, cc * 128:(cc + 1) * 128], ("hn", mt),
                              pb, mt, hT, ("hT",), mt * 128)
            nb = [0]

            def nextpf0():
                bi = nb[0] % 6
                nb[0] += 1
                return bi
            for fc in range(8):
                bi = nextpf0()
                for cc in range(8):
                    sch.op("pe", lambda e, cc=cc, fc=fc, bi=bi: e.matmul(
                        pf[bi][:, 0:256], lhsT=wckv[:, cc, fc * 128:(fc + 1) * 128], rhs=hT[:, cc, 0:256],
                        start=(cc == 0), stop=(cc == 7)), r=[("wckv", fc // 2), ("hT",)], w=[("pf", bi)])
                sch.op("dve", lambda e, fc=fc, bi=bi: e.tensor_copy(out=kcT[:, fc, :], in_=pf[bi][:, 0:256]),
                       r=[("pf", bi)], w=[("kcT",)])
            for mt in range(2):
                for half in range(2):
                    bi = nextpf0()
                    for cc in range(8):
                        sch.op("pe", lambda e, cc=cc, mt=mt, half=half, bi=bi: e.matmul(
                            pf[bi][:, :], lhsT=hT[:, cc, mt * 128:(mt + 1) * 128],
                            rhs=wckv[:, cc, D + half * 512:D + (half + 1) * 512],
                            start=(cc == 0), stop=(cc == 7)),
                               r=[("wckv", 4 + half * 2), ("wckv", 5 + half * 2), ("hT",)], w=[("pf", bi)])
                    sch.op("dve", lambda e, mt=mt, half=half, bi=bi: e.tensor_copy(
                        out=vc[:, mt, half * 512:(half + 1) * 512], in_=pf[bi][:, :]),
                           r=[("pf", bi)], w=[("vc",)])
            sch.emit_stage()

        with ExitStack() as es:
            T = lambda n, shp, dt: es.enter_context(nc.sbuf_tensor("cr" + n, shp, dt))
            wout = T("wout", [128, 8, D], BF16)
            wcq = T("wcq", [128, 8, D], BF16)
            wco = T("wco", [128, 8, D], BF16)
            gO = T("gO", [128, D], F32)
            gC = T("gC", [128, D], F32)
            ot = T("ot", [128, 2, D], F32)
            x1t = T("x1t", [128, 4, D], F32)
            x2 = T("x2", [128, 2, 4, D], F32)
            hn = T("hn", [128, 4, D], BF16)
            yT = T("yT", [128, 8, 512], BF16)
            hT = T("hT", [128, 2, 8, 512], BF16)
            qcT = T("qcT", [128, 8, 512], BF16)
            Pc = T("Pc", [128, 2, 2, 512], BF16)
            rdt = T("rdt", [128, 2, 512], F32)
            ocT = T("ocT", [128, 8, 512], BF16)
            pf, pb = _psum_banks(nc, es, 6, 2)
            cnt = {"pf": 0, "k": 0}

            def nextpf():
                bi = cnt["pf"] % 6
                cnt["pf"] += 1
                return bi

            _load_gain(sch, C, gO, 2, "O")
            _load_gain(sch, C, gC, 3, "C")

            def load_w():
                ki = 0
                for (wt, wd, nm) in ((wout, C.w_out, "wout"), (wcq, C.w_cq, "wcq"), (wco, C.w_co, "wco")):
                    for j in range(4):
                        _load_weight_cols(sch, wt, wd, j * 256, (j + 1) * 256, (nm, j), ki); ki += 1

            n1k = {}

            def P1_norm(ci):
                ks = []
                for s in range(4):
                    t = ci * 4 + s
                    sl = t % 2
                    sch.op("sp", lambda e, sl=sl, t=t: e.dma_start(out=ot[:, sl, :], in_=C.O[t]),
                           w=[("ot", sl)], dma="ol%d" % sl)
                    sch.op("sp", lambda e, s=s, t=t: e.dma_start(out=x1t[:, s, :],
                                                                in_=C.x1[t * 128:(t + 1) * 128, :]),
                           w=[("x1t", s)], dma="xl%d" % s)
                    k = cnt["k"]; cnt["k"] += 1
                    ks.append(k)
                    b = (k % 4) * 3
                    st = C.st
                    sa, sb, sc = st[:, b:b + 1], st[:, b + 1:b + 2], st[:, b + 2:b + 3]
                    b2 = 12 + (k % 4) * 3
                    sa2, sb2, sc2 = st[:, b2:b2 + 1], st[:, b2 + 1:b2 + 2], st[:, b2 + 2:b2 + 3]
                    tk = ("st", k % 4)
                    kk = k % 4
                    sch.op("act", lambda e, sl=sl, kk=kk, sa=sa: e.activation(
                        out=hn[:, kk, 0:768], in_=ot[:, sl, 0:768], func=AF.Square, accum_out=sa),
                           r=[("ot", sl)], w=[("hn", kk), tk])
                    sch.op("act", lambda e, sl=sl, kk=kk, sa2=sa2: e.activation(
                        out=hn[:, kk, 768:1024], in_=ot[:, sl, 768:1024], func=AF.Square, accum_out=sa2),
                           r=[("ot", sl)], w=[("hn", kk), tk])
                    sch.op("dve", lambda e, sa=sa, sb=sb: e.tensor_scalar(
                        out=sb, in0=sa, scalar1=1.0 / 768, scalar2=EPS, op0=ALU.mult, op1=ALU.add), r=[tk], w=[tk])
                    sch.op("dve", lambda e, sa2=sa2, sb2=sb2: e.tensor_scalar(
                        out=sb2, in0=sa2, scalar1=1.0 / 256, scalar2=EPS, op0=ALU.mult, op1=ALU.add), r=[tk], w=[tk])
                    sch.op("pool", lambda e, sb=sb, sc=sc: e.tensor_tensor(
                        out=sc, in0=sb, in1=C.mhalf[:, 0:1], op=ALU.pow), r=[tk, ("mhalf",)], w=[tk])
                    sch.op("pool", lambda e, sb2=sb2, sc2=sc2: e.tensor_tensor(
                        out=sc2, in0=sb2, in1=C.mhalf[:, 0:1], op=ALU.pow), r=[tk, ("mhalf",)], w=[tk])
                    sch.op("dve", lambda e, sl=sl, kk=kk, sc=sc: e.scalar_tensor_tensor(
                        out=hn[:, kk, 0:768], in0=ot[:, sl, 0:768], scalar=sc, in1=gO[:, 0:768],
                        op0=ALU.mult, op1=ALU.mult), r=[("ot", sl), tk, ("g", "O")], w=[("hn", kk)])
                    sch.op("dve", lambda e, sl=sl, kk=kk, sc2=sc2: e.scalar_tensor_tensor(
                        out=hn[:, kk, 768:1024], in0=ot[:, sl, 768:1024], scalar=sc2, in1=gO[:, 768:1024],
                        op0=ALU.mult, op1=ALU.mult), r=[("ot", sl), tk, ("g", "O")], w=[("hn", kk)])
                    n1k[(ci, s)] = k

            def P1_tr(ci):
                for s in range(4):
                    k = n1k[(ci, s)]
                    kk = k % 4
                    _transpose_to(sch, C, lambda cc, kk=kk: hn[:, kk, cc * 128:(cc + 1) * 128], ("hn", kk),
                                  pb, k % 2, yT, ("yT", s), s * 128)

            def P1_wout(ci):
                xs = ci % 2
                for s in range(4):
                    for half in range(2):
                        bi = nextpf()
                        for cc in range(8):
                            sch.op("pe", lambda e, cc=cc, s=s, half=half, bi=bi: e.matmul(
                                pf[bi][:, :], lhsT=yT[:, cc, s * 128:(s + 1) * 128],
                                rhs=wout[:, cc, half * 512:(half + 1) * 512], start=(cc == 0), stop=(cc == 7)),
                                   r=[("yT", s), ("wout", half * 2), ("wout", half * 2 + 1)], w=[("pf", bi)])
                        sch.op("dve", lambda e, s=s, xs=xs, half=half, bi=bi: e.tensor_tensor(
                            out=x2[:, xs, s, half * 512:(half + 1) * 512], in0=pf[bi][:, :],
                            in1=x1t[:, s, half * 512:(half + 1) * 512], op=ALU.add),
                               r=[("pf", bi), ("x1t", s)], w=[("x2", xs, s)])

            n2k = {}

            def P1_norm2(ci):
                xs = ci % 2
                for s in range(4):
                    k = cnt["k"]; cnt["k"] += 1
                    n2k[(ci, s)] = k
                    kk = k % 4
                    _norm_to_hn(sch, C, x2[:, xs, s, :], ("x2", xs, s), gC, "C", hn[:, kk, :], ("hn", kk), k)

            def P1_tr2(ci):
                xs = ci % 2
                for s in range(4):
                    k = n2k[(ci, s)]
                    kk = k % 4
                    _transpose_to(sch, C, lambda cc, kk=kk: hn[:, kk, cc * 128:(cc + 1) * 128], ("hn", kk),
                                  pb, k % 2, hT[:, xs], ("hT", xs), s * 128)

            def P2(ci):
                xs = ci % 2
                for fc in range(8):
                    bi = nextpf()
                    for cc in range(8):
                        sch.op("pe", lambda e, cc=cc, fc=fc, bi=bi, xs=xs: e.matmul(
                            pf[bi][:, :], lhsT=wcq[:, cc, fc * 128:(fc + 1) * 128], rhs=hT[:, xs, cc, :],
                            start=(cc == 0), stop=(cc == 7)), r=[("wcq", fc // 2), ("hT", xs)], w=[("pf", bi)])
                    sch.op("act", lambda e, fc=fc, bi=bi: e.copy(out=qcT[:, fc, :], in_=pf[bi][:, :]),
                           r=[("pf", bi)], w=[("qcT", fc)])

            def P3(ci):
                for hd in range(4):
                    pslot = hd % 2
                    for mt in range(2):
                        bi = nextpf()
                        for e2 in range(2):
                            sch.op("pe", lambda e, e2=e2, mt=mt, hd=hd, bi=bi: e.matmul(
                                pf[bi][:, :], lhsT=kcT[:, 2 * hd + e2, mt * 128:(mt + 1) * 128],
                                rhs=qcT[:, 2 * hd + e2, :], start=(e2 == 0), stop=(e2 == 1)),
                                   r=[("kcT",), ("qcT", 2 * hd + e2)], w=[("pf", bi)])
                        sch.op("act", lambda e, mt=mt, pslot=pslot, bi=bi: e.activation(
                            out=Pc[:, pslot, mt, :], in_=pf[bi][:, :], func=AF.Exp, scale=1.0 / 16.0),
                               r=[("pf", bi)], w=[("Pc", pslot, mt)])
                    bd = nextpf()
                    for mt in range(2):
                        sch.op("pe", lambda e, mt=mt, pslot=pslot, bd=bd: e.matmul(
                            pf[bd][:, :], lhsT=C.ones[:, :], rhs=Pc[:, pslot, mt, :],
                            start=(mt == 0), stop=(mt == 1)),
                               r=[("ones",), ("Pc", pslot, mt)], w=[("pf", bd)])
                    sch.op("dve", lambda e, pslot=pslot, bd=bd: e.reciprocal(out=rdt[:, pslot, :], in_=pf[bd][:, :]),
                           r=[("pf", bd)], w=[("rdt", pslot)])
                    for e2 in range(2):
                        bi = nextpf()
                        for mt in range(2):
                            sch.op("pe", lambda e, e2=e2, mt=mt, hd=hd, pslot=pslot, bi=bi: e.matmul(
                                pf[bi][:, :], lhsT=vc[:, mt, (2 * hd + e2) * 128:(2 * hd + e2 + 1) * 128],
                                rhs=Pc[:, pslot, mt, :], start=(mt == 0), stop=(mt == 1)),
                                   r=[("vc",), ("Pc", pslot, mt)], w=[("pf", bi)])
                        sch.op("dve", lambda e, e2=e2, hd=hd, pslot=pslot, bi=bi: e.tensor_tensor(
                            out=ocT[:, 2 * hd + e2, :], in0=pf[bi][:, :], in1=rdt[:, pslot, :], op=ALU.mult),
                               r=[("pf", bi), ("rdt", pslot)], w=[("ocT", 2 * hd + e2)])

            def P4(ci):
                xs = ci % 2
                for s in range(4):
                    t = ci * 4 + s
                    for half in range(2):
                        bi = nextpf()
                        for cc in range(8):
                            sch.op("pe", lambda e, cc=cc, s=s, half=half, bi=bi: e.matmul(
                                pf[bi][:, :], lhsT=ocT[:, cc, s * 128:(s + 1) * 128],
                                rhs=wco[:, cc, half * 512:(half + 1) * 512], start=(cc == 0), stop=(cc == 7)),
                                   r=[("ocT", cc), ("wco", half * 2), ("wco", half * 2 + 1)], w=[("pf", bi)])
                        sch.op("dve", lambda e, s=s, xs=xs, half=half, bi=bi: e.tensor_tensor(
                            out=x2[:, xs, s, half * 512:(half + 1) * 512], in0=pf[bi][:, :],
                            in1=x2[:, xs, s, half * 512:(half + 1) * 512], op=ALU.add),
                               r=[("pf", bi), ("x2", xs, s)], w=[("x2", xs, s)])
                    sch.op("sp", lambda e, s=s, xs=xs, t=t: e.dma_start(out=C.x3[t * 128:(t + 1) * 128, :],
                                                                       in_=x2[:, xs, s, :]),
                           r=[("x2", xs, s)], w=[("x3", t)], dma="xs%d%d" % (xs, s))

            P1_norm(0)
            load_w()
            P1_tr(0)
            P1_wout(0)
            P1_norm2(0)
            P1_tr2(0)
            for ci in range(8):
                if ci + 1 < 8:
                    P1_norm(ci + 1)
                P2(ci)
                if ci + 1 < 8:
                    P1_tr(ci + 1)
                P3(ci)
                if ci + 1 < 8:
                    P1_wout(ci + 1)
                    P1_norm2(ci + 1)
                P4(ci)
                if ci + 1 < 8:
                    P1_tr2(ci + 1)
            sch.emit_stage()


def build_program(debug=False, upto=5):
    nc = bass.Bass("TRN2", target_bir_lowering=False)
    C = Ctx()

    def din(name, shape, dt=F32):
        return nc.dram_tensor(name, shape, dt, kind="ExternalInput")

    def dscr(name, shape, dt):
        return nc.dram_tensor(name, shape, dt, kind=("ExternalOutput" if debug else "Internal"))

    C.x = din("x", [S, D]).ap()
    C.mem = din("mem", [256, D]).ap()
    C.w_ffn1_gu = din("w_ffn1_gu", [D, 2 * DFF]).ap()
    C.w_ffn1_down = din("w_ffn1_down", [DFF, D]).ap()
    C.w_in = din("w_in", [D, 3 * D]).ap()
    C.w_out = din("w_out", [D, D]).ap()
    C.w_cq = din("w_cq", [D, D]).ap()
    C.w_ckv = din("w_ckv", [D, 2 * D]).ap()
    C.w_co = din("w_co", [D, D]).ap()
    C.w_ffn2_gu = din("w_ffn2_gu", [D, 2 * DFF]).ap()
    C.w_ffn2_down = din("w_ffn2_down", [DFF, D]).ap()
    C.gall_t = din("gall", [7, D])
    C.tb = din("tb", [4, 128, NW]).ap()
    C.cmask = din("cmask", [128, NW]).ap()
    C.tbs = din("tbs", [12, 128, 3, 256]).ap()
    C.cms = din("cms", [128, 256]).ap()
    C.kbind = din("kbind", [16, S]).ap()
    C.mconst = din("mconst", [128, 16, 16]).ap()
    C.identf = din("identf", [128, 128]).ap()
    C.out = nc.dram_tensor("out", [S, D], F32, kind="ExternalOutput").ap()
    C.x1 = dscr("x1", [S, D], F32).ap()
    C.qT = dscr("qT", [D, S], BF16).ap()
    C.kT = dscr("kT", [D, S], BF16).ap()
    C.vT = dscr("vT", [768, S], BF16).ap()
    C.V = dscr("V", [4, 128, NT, 65], BF16).ap()
    C.O = dscr("O", [NT, 128, D], F32).ap()
    C.x3 = dscr("x3", [S, D], F32).ap()

    with ExitStack() as es:
        C.ident = es.enter_context(nc.sbuf_tensor("ident", [128, 128], BF16))
        C.ones = es.enter_context(nc.sbuf_tensor("ones", [128, 128], BF16))
        C.mhalf = es.enter_context(nc.sbuf_tensor("mhalf", [128, 1], F32))
        C.st = es.enter_context(nc.sbuf_tensor("st", [128, 24], F32))
        sch = Sched(nc, es)
        sch.op("pool", lambda e: e.dma_start(out=C.ident[:, :], in_=C.identf), w=[("ident",)], dma="w0")
        sch.op("pool", lambda e: e.memset(C.ones[:, :], 1.0), w=[("ones",)])
        sch.op("pool", lambda e: e.memset(C.mhalf[:, :], -0.5), w=[("mhalf",)])
        sch.emit_stage()

        if upto >= 1:
            ffn_stage(nc, sch, C, "f1", C.x, C.x1, C.w_ffn1_gu, C.w_ffn1_down, 0, None)
        if upto >= 2:
            inproj_stage(nc, sch, C)
        if upto >= 3:
            attn_stage(nc, sch, C)
        if upto >= 4:
            cross_stage(nc, sch, C)
        if upto >= 5:
            ffn_stage(nc, sch, C, "f2", C.x3, C.out, C.w_ffn2_gu, C.w_ffn2_down, 5, 6)
        sch.final_wait()
    return nc


def _rel_bucket_np(dist):
    n = np.maximum(dist, 0)
    nf = np.maximum(n, 1).astype(np.float32)
    large = 16 + (np.log(nf / np.float32(16.0)) / np.float32(math.log(2048 / 16)) * np.float32(16)).astype(np.int32)
    large = np.minimum(large, 31)
    return np.where(n < 16, n, large)


def _host_consts(rel_bias):
    p = np.arange(128)[:, None]
    j = np.arange(NW)[None, :]
    Dm = j - WOFF - p
    bk = _rel_bucket_np(Dm)
    tb = np.ascontiguousarray(np.transpose(rel_bias[bk, 12:16], (2, 0, 1))).astype(np.float32)
    cmask = (Dm >= 0).astype(np.float32)
    c = np.arange(256)[None, :]
    dsub = np.maximum(c - p, 0)
    tbs = np.zeros((12, 128, 3, 256), np.float32)
    for di, d in enumerate((1, 4, 16)):
        bks = _rel_bucket_np(dsub * d)
        tbs[:, :, di, :] = np.transpose(rel_bias[bks, 0:12], (2, 0, 1))
    cms = (((c - p) >= 0) & ((c - p) <= 128)).astype(np.float32)
    kbind = np.zeros((16, S), np.float32)
    for jb in range(16):
        kbind[jb, jb * 256:(jb + 1) * 256] = 30000.0
    mconst = np.zeros((128, 16, 16), np.float32)
    for n in range(16):
        mconst[:, n, n] = 1e30
        mconst[:, n, n + 1:] = -1e30
    ident = np.eye(128, dtype=np.float32)
    return tb, cmask, tbs, cms, kbind, mconst, ident


_CACHE = {}


def _get_program(debug=False, upto=5):
    key = (debug, upto)
    if key not in _CACHE:
        _CACHE[key] = build_program(debug, upto)
    return _CACHE[key]


def _in_maps(inputs):
    f = lambda a: np.ascontiguousarray(np.asarray(a, dtype=np.float32))
    x = f(inputs["x"])
    mem = f(inputs["mem"])
    rel_bias = f(inputs["rel_bias"])
    tb, cmask, tbs, cms, kbind, mconst, ident = _host_consts(rel_bias)
    gall = np.stack([
        f(inputs["g_ffn1"])[0], f(inputs["g_mix"])[0],
        np.concatenate([f(inputs["g_out_a"])[0], f(inputs["g_out_b"])[0]]),
        f(inputs["g_cross"])[0], f(inputs["g_mem"])[0], f(inputs["g_ffn2"])[0], f(inputs["g_final"]),
    ]).astype(np.float32)
    shared = {
        "w_ffn1_gu": f(inputs["w_ffn1_gu"])[0], "w_ffn1_down": f(inputs["w_ffn1_down"])[0],
        "w_in": f(inputs["w_in"])[0], "w_out": f(inputs["w_out"])[0], "w_cq": f(inputs["w_cq"])[0],
        "w_ckv": f(inputs["w_ckv"])[0], "w_co": f(inputs["w_co"])[0],
        "w_ffn2_gu": f(inputs["w_ffn2_gu"])[0], "w_ffn2_down": f(inputs["w_ffn2_down"])[0],
        "gall": gall, "tb": tb, "cmask": cmask, "tbs": tbs, "cms": cms, "kbind": kbind, "mconst": mconst, "identf": ident,
    }
    maps = []
    for b in range(NCORES):
        m = dict(shared)
        m["x"] = np.ascontiguousarray(x[b])
        m["mem"] = np.ascontiguousarray(mem[b])
        maps.append(m)
    return maps


def kernel(**inputs):
    nc = _get_program(False, 5)
    maps = _in_maps(inputs)
    res = run_bass_kernel_spmd(nc, maps, core_ids=list(range(NCORES)))
    out = np.stack([np.asarray(r["out"], dtype=np.float32).reshape(S, D) for r in res.results], axis=0)
    return out
```
